# Optimizing a Trainium2 kernel written in Bass

```python
import math
import jax, jax.numpy as jnp
from jax import lax
import numpy as np

D_MODEL = 1024
BATCH = 4
SEQ = 4096
DEPTH = 2

GRID_W = 64
CTX_LEN = 256
N_MIXERS = 2
MIXER_GDN = 0
MIXER_POOL = 1
N_DIRS = 2

GDN_HEADS = 8
GDN_HEAD_DIM = 128
GDN_KEY_DIM = GDN_HEADS * GDN_HEAD_DIM
GDN_VALUE_DIM = GDN_HEADS * GDN_HEAD_DIM
GDN_QKV_DIM = 2 * GDN_KEY_DIM + GDN_VALUE_DIM
GDN_AB_DIM = N_DIRS * 2 * GDN_HEADS
GDN_PROJ_DIM = GDN_QKV_DIM + GDN_VALUE_DIM + GDN_AB_DIM
CONV_W = 5
CHUNK = 64

POOL_WINDOWS = (2, 4, 8, 16)
POOL_GROUPS = len(POOL_WINDOWS)
POOL_GROUP_DIM = D_MODEL // POOL_GROUPS

N_EXPERTS = 16
EC_CAPACITY_FACTOR = 2
D_EXPERT = 2048

N_MOD = 6
RMS_EPS = 1e-6

kernel_name = 'bidir_gdn_pool_ec_moe_dit'


def rmsnorm(x, g):
    xf = x.astype(jnp.float32)
    y = xf * lax.rsqrt(jnp.mean(xf * xf, axis=-1, keepdims=True) + RMS_EPS)
    return (y * g.astype(jnp.float32)).astype(x.dtype)


def l2norm(x):
    xf = x.astype(jnp.float32)
    return xf * lax.rsqrt(jnp.sum(xf * xf, axis=-1, keepdims=True) + RMS_EPS)


def modulate(xn, shift, scale):
    return xn * (1 + scale) + shift


def short_conv(x, w):
    pad = CONV_W // 2
    return lax.conv_general_dilated(x, w[:, None, :], window_strides=(1,), padding=[(pad, pad)],
                                    dimension_numbers=('NWC', 'WIO', 'NWC'),
                                    feature_group_count=x.shape[-1])


def chunk_gated_delta(q, k, v, g, beta, s0):
    f32 = jnp.float32
    B, H, T, DK = q.shape
    DV = v.shape[-1]
    n = T // CHUNK
    q = q.astype(f32).reshape(B, H, n, CHUNK, DK)
    k = k.astype(f32).reshape(B, H, n, CHUNK, DK)
    v = v.astype(f32).reshape(B, H, n, CHUNK, DV)
    g = g.astype(f32).reshape(B, H, n, CHUNK)
    beta = beta.astype(f32).reshape(B, H, n, CHUNK)
    gcum = jnp.cumsum(g, axis=-1)
    incl = jnp.tril(jnp.ones((CHUNK, CHUNK), dtype=bool))
    strict = jnp.tril(jnp.ones((CHUNK, CHUNK), dtype=bool), -1)
    diff = gcum[..., :, None] - gcum[..., None, :]
    decay = jnp.where(incl, jnp.exp(jnp.where(incl, diff, 0.0)), 0.0)
    k_beta = k * beta[..., None]
    v_beta = v * beta[..., None]
    a_low = jnp.where(strict, jnp.einsum('bhnid,bhnjd->bhnij', k_beta, k) * decay, 0.0)
    lmat = a_low + jnp.eye(CHUNK, dtype=f32)
    rhs = jnp.concatenate([v_beta, k_beta * jnp.exp(gcum)[..., None]], axis=-1)
    sol = lax.linalg.triangular_solve(lmat, rhs, left_side=True, lower=True, unit_diagonal=True)
    u_vals, w_keys = sol[..., :DV], sol[..., DV:]
    att = jnp.einsum('bhnid,bhnjd->bhnij', q, k) * decay
    q_dec = q * jnp.exp(gcum)[..., None]
    k_tail = k * jnp.exp(gcum[..., -1:] - gcum)[..., None]
    g_tail = jnp.exp(gcum[..., -1])
    xs = tuple(jnp.moveaxis(t, 2, 0) for t in (u_vals, w_keys, q_dec, att, k_tail, g_tail))

    def step(S, inp):
        u_n, w_n, qd_n, att_n, kt_n, gt_n = inp
        v_new = u_n - jnp.einsum('bhck,bhkv->bhcv', w_n, S)
        o_n = jnp.einsum('bhck,bhkv->bhcv', qd_n, S) + jnp.einsum('bhij,bhjv->bhiv', att_n, v_new)
        S = S * gt_n[..., None, None] + jnp.einsum('bhck,bhcv->bhkv', kt_n, v_new)
        return S, o_n

    s_fin, o = lax.scan(step, s0.astype(f32), xs)
    o = jnp.moveaxis(o, 0, 2).reshape(B, H, T, DV)
    return o, s_fin


def gdn_project(u, w_in, conv_w, a_log, dt_bias):
    B, T, _ = u.shape
    p = u @ w_in
    qkv = jax.nn.silu(short_conv(p[..., :GDN_QKV_DIM], conv_w))
    z = p[..., GDN_QKV_DIM:GDN_QKV_DIM + GDN_VALUE_DIM]
    ab = p[..., GDN_QKV_DIM + GDN_VALUE_DIM:].astype(jnp.float32).reshape(B, T, N_DIRS, 2, GDN_HEADS)
    q = qkv[..., :GDN_KEY_DIM]
    k = qkv[..., GDN_KEY_DIM:2 * GDN_KEY_DIM]
    v = qkv[..., 2 * GDN_KEY_DIM:]
    heads = lambda t: t.reshape(B, T, GDN_HEADS, GDN_HEAD_DIM).transpose(0, 2, 1, 3)
    q = l2norm(heads(q)) * (GDN_HEAD_DIM ** -0.5)
    k = l2norm(heads(k))
    v = heads(v)
    a = ab[:, :, :, 0, :].transpose(2, 0, 3, 1)
    b = ab[:, :, :, 1, :].transpose(2, 0, 3, 1)
    g = -jnp.exp(a_log.astype(jnp.float32))[:, None, :, None] * jax.nn.softplus(
        a + dt_bias.astype(jnp.float32)[:, None, :, None])
    beta = jax.nn.sigmoid(b)
    return q, k, v, z, g, beta


def gdn_out(o, z, o_gain, w_out):
    B, H, T, DV = o.shape
    o = o.transpose(0, 2, 1, 3)
    zf = z.reshape(B, T, H, DV).astype(jnp.float32)
    o = o * lax.rsqrt(jnp.mean(o * o, axis=-1, keepdims=True) + RMS_EPS) * o_gain.astype(jnp.float32) * jax.nn.silu(zf)
    return o.reshape(B, T, H * DV).astype(z.dtype) @ w_out


def gated_deltanet(u, uc, w_in, conv_w, a_log, dt_bias, o_gain, w_out, with_ctx_out):
    ql, kl, vl, zl, gl, bl = gdn_project(u, w_in, conv_w, a_log, dt_bias)
    qc, kc, vc, zc, gc, bc = gdn_project(uc, w_in, conv_w, a_log, dt_bias)
    B = u.shape[0]
    o_lat = 0.0
    o_ctx = 0.0
    for d in range(N_DIRS):
        flip = (lambda t: jnp.flip(t, axis=2)) if d == 1 else (lambda t: t)
        s0 = jnp.zeros((B, GDN_HEADS, GDN_HEAD_DIM, GDN_HEAD_DIM), jnp.float32)
        oc, s_ctx = chunk_gated_delta(flip(qc), flip(kc), flip(vc), flip(gc[d]), flip(bc[d]), s0)
        ol, _ = chunk_gated_delta(flip(ql), flip(kl), flip(vl), flip(gl[d]), flip(bl[d]), s_ctx)
        o_lat = o_lat + flip(ol)
        if with_ctx_out:
            o_ctx = o_ctx + flip(oc)
    y = gdn_out(o_lat, zl, o_gain, w_out)
    yc = gdn_out(o_ctx, zc, o_gain, w_out) if with_ctx_out else None
    return y, yc


def window_mean(x, w, axis):
    n = x.shape[axis]
    cs = jnp.cumsum(x.astype(jnp.float32), axis=axis)
    pad_width = [(0, 0)] * x.ndim
    pad_width[axis] = (1, 0)
    cs = jnp.pad(cs, pad_width)
    pos = jnp.arange(n)
    lo = jnp.clip(pos - w // 2, 0, n)
    hi = jnp.clip(pos + (w - w // 2), 0, n)
    total = jnp.take(cs, hi, axis=axis) - jnp.take(cs, lo, axis=axis)
    shape = [1] * x.ndim
    shape[axis] = n
    count = (hi - lo).astype(jnp.float32).reshape(shape)
    return (total / count).astype(x.dtype)


def pool_mixer(u, w_grp, scale, on_grid):
    B, T, D = u.shape
    if on_grid:
        rows = T // GRID_W
        xg = u.reshape(B, rows, GRID_W, POOL_GROUPS, POOL_GROUP_DIM)
        axes = (2, 1)
    else:
        xg = u.reshape(B, T, POOL_GROUPS, POOL_GROUP_DIM)
        axes = (1,)
    diffs = []
    for gi, w in enumerate(POOL_WINDOWS):
        xi = xg[..., gi, :]
        m = xi
        for ax in axes:
            m = window_mean(m, w, ax)
        diffs.append(m - xi)
    p = jnp.stack(diffs, axis=-2)
    y = jnp.einsum('...gc,gce->...ge', p, w_grp).reshape(B, T, D)
    return y * scale


def ec_moe(u, w_router, w_gate, w_up, w_down):
    B, T, D = u.shape
    cap = (EC_CAPACITY_FACTOR * T) // N_EXPERTS
    aff = jax.nn.softmax((u @ w_router).astype(jnp.float32), axis=-1)
    gates, idx = lax.top_k(jnp.swapaxes(aff, 1, 2), cap)
    bidx = jnp.arange(B)[:, None, None]
    xs = u[bidx, idx]
    hid = jax.nn.silu(jnp.einsum('becd,edf->becf', xs, w_gate)) * jnp.einsum('becd,edf->becf', xs, w_up)
    ys = jnp.einsum('becf,efd->becd', hid, w_down) * gates[..., None].astype(u.dtype)
    return jnp.zeros_like(u).at[bidx, idx].add(ys)


def setup_inputs(seed: int = 0) -> dict:
    key = jax.random.key(seed)
    ks = jax.random.split(key, 24)
    f32 = jnp.float32
    n_gdn = len(range(MIXER_GDN, DEPTH, N_MIXERS))
    n_pool = len(range(MIXER_POOL, DEPTH, N_MIXERS))
    nrm = lambda k, shape, s: jax.random.normal(k, shape, f32) * s
    dt = jnp.exp(jax.random.uniform(ks[10], (n_gdn, N_DIRS, GDN_HEADS), f32,
                                    minval=math.log(1e-3), maxval=math.log(1e-1)))
    return {
        'x': nrm(ks[0], (BATCH, SEQ, D_MODEL), 1.0),
        'c': nrm(ks[1], (BATCH, D_MODEL), 1.0),
        'ctx': nrm(ks[2], (BATCH, CTX_LEN, D_MODEL), 1.0),
        'c_ctx': nrm(ks[3], (D_MODEL,), 1.0),
        'w_mod': nrm(ks[4], (DEPTH, D_MODEL, N_MOD * D_MODEL), 0.3 * D_MODEL ** -0.5),
        'b_mod': nrm(ks[5], (DEPTH, N_MOD * D_MODEL), 0.02),
        'norm_mix_g': 1.0 + nrm(ks[6], (DEPTH, D_MODEL), 0.02),
        'norm_ffn_g': 1.0 + nrm(ks[7], (DEPTH, D_MODEL), 0.02),
        'gdn_w_in': nrm(ks[8], (n_gdn, D_MODEL, GDN_PROJ_DIM), D_MODEL ** -0.5),
        'gdn_conv_w': nrm(ks[9], (n_gdn, CONV_W, GDN_QKV_DIM), CONV_W ** -0.5),
        'gdn_a_log': jnp.log(jax.random.uniform(ks[11], (n_gdn, N_DIRS, GDN_HEADS), f32, minval=1.0, maxval=16.0)),
        'gdn_dt_bias': dt + jnp.log(-jnp.expm1(-dt)),
        'gdn_o_gain': 1.0 + nrm(ks[12], (n_gdn, GDN_HEAD_DIM), 0.02),
        'gdn_w_out': nrm(ks[13], (n_gdn, GDN_VALUE_DIM, D_MODEL), GDN_VALUE_DIM ** -0.5),
        'pool_w': nrm(ks[14], (n_pool, POOL_GROUPS, POOL_GROUP_DIM, POOL_GROUP_DIM), POOL_GROUP_DIM ** -0.5),
        'pool_scale': 1.0 + nrm(ks[15], (n_pool, D_MODEL), 0.05),
        'moe_w_router': nrm(ks[16], (DEPTH, D_MODEL, N_EXPERTS), D_MODEL ** -0.5),
        'moe_w_gate': nrm(ks[17], (DEPTH, N_EXPERTS, D_MODEL, D_EXPERT), D_MODEL ** -0.5),
        'moe_w_up': nrm(ks[18], (DEPTH, N_EXPERTS, D_MODEL, D_EXPERT), D_MODEL ** -0.5),
        'moe_w_down': nrm(ks[19], (DEPTH, N_EXPERTS, D_EXPERT, D_MODEL), D_EXPERT ** -0.5),
        'final_g': 1.0 + nrm(ks[20], (D_MODEL,), 0.02),
    }


def reference(x, c, ctx, c_ctx, w_mod, b_mod, norm_mix_g, norm_ffn_g,
              gdn_w_in, gdn_conv_w, gdn_a_log, gdn_dt_bias, gdn_o_gain, gdn_w_out,
              pool_w, pool_scale, moe_w_router, moe_w_gate, moe_w_up, moe_w_down, final_g):
    h, hc = x, ctx
    c_act = jax.nn.silu(c)
    cc_act = jax.nn.silu(c_ctx)
    for i in range(DEPTH):
        kind = i % N_MIXERS
        j = i // N_MIXERS
        ctx_carry = any(l % N_MIXERS == MIXER_GDN for l in range(i + 1, DEPTH))
        mod = c_act @ w_mod[i] + b_mod[i]
        sh_m, sc_m, gt_m, sh_f, sc_f, gt_f = [t[:, None, :] for t in jnp.split(mod, N_MOD, axis=-1)]
        u = modulate(rmsnorm(h, norm_mix_g[i]), sh_m, sc_m)
        uc = None
        mod_c = None
        if kind == MIXER_GDN or ctx_carry:
            mod_c = jnp.split(cc_act @ w_mod[i] + b_mod[i], N_MOD, axis=-1)
            uc = modulate(rmsnorm(hc, norm_mix_g[i]), mod_c[0], mod_c[1])
        if kind == MIXER_GDN:
            y, yc = gated_deltanet(u, uc, gdn_w_in[j], gdn_conv_w[j], gdn_a_log[j], gdn_dt_bias[j],
                                   gdn_o_gain[j], gdn_w_out[j], ctx_carry)
        else:
            y = pool_mixer(u, pool_w[j], pool_scale[j], True)
            yc = pool_mixer(uc, pool_w[j], pool_scale[j], False) if ctx_carry else None
        h = h + gt_m * y
        h = h + gt_f * ec_moe(modulate(rmsnorm(h, norm_ffn_g[i]), sh_f, sc_f),
                              moe_w_router[i], moe_w_gate[i], moe_w_up[i], moe_w_down[i])
        if ctx_carry:
            hc = hc + mod_c[2] * yc
            hc = hc + mod_c[5] * ec_moe(modulate(rmsnorm(hc, norm_ffn_g[i]), mod_c[3], mod_c[4]),
                                        moe_w_router[i], moe_w_gate[i], moe_w_up[i], moe_w_down[i])
    return rmsnorm(h, final_g)
```

```python
import ml_dtypes
from concourse.bass_utils import run_bass_kernel_spmd
from contextlib import ExitStack
import numpy as np
import concourse.bass as bass
import concourse.mybir as mybir

F32 = mybir.dt.float32
BF16 = mybir.dt.bfloat16
I32 = mybir.dt.int32
U32 = mybir.dt.uint32
AF = mybir.ActivationFunctionType
ALU = mybir.AluOpType
AX = mybir.AxisListType


class Tk:
    __slots__ = ("w", "r", "rd")

    def __init__(self):
        self.w = None
        self.r = {}
        self.rd = []


class Op:
    __slots__ = ("fn", "deps", "dma", "signal", "dmaid")

    def __init__(self, fn, deps, dma):
        self.fn = fn
        self.deps = deps
        self.dma = dma
        self.signal = False
        self.dmaid = None


class Prog:
    ENG = ("pe", "act", "dve", "pool", "sp")
    NDS = 8
    _uid = 0
    _g = {}
    _tn = 0

    def __init__(self, nc):
        self.nc = nc
        self.es = ExitStack()
        self.ops = {e: [] for e in self.ENG}
        self.ndma = {e: 0 for e in self.ENG}
        self.dma_of = {}
        self._n = 0

    def sb(self, shape, dtype, name=None):
        Prog._tn += 1
        name = name or f"sb{Prog._tn}"
        return self.es.enter_context(self.nc.sbuf_tensor(name, list(shape), dtype))

    def ps(self, shape, dtype=F32, name=None):
        Prog._tn += 1
        name = name or f"ps{Prog._tn}"
        return self.es.enter_context(self.nc.psum_tensor(name, list(shape), dtype))

    def tk(self):
        return Tk()

    def op(self, eng, fn, reads=(), writes=(), dma=False):
        idx = len(self.ops[eng])
        me = ("dma", eng, self.ndma[eng]) if dma else (eng, idx)
        deps = set()
        for t in reads:
            if t.w is not None:
                deps.add(t.w)
        for t in writes:
            if t.w is not None:
                deps.add(t.w)
            for e2, j in t.r.items():
                deps.add((e2, j))
            for d in t.rd:
                deps.add(d)
        for t in reads:
            if dma:
                t.rd.append(me)
            else:
                if t.r.get(eng, -1) < idx:
                    t.r[eng] = idx
        for t in writes:
            t.w = me
            t.r = {}
            t.rd = []
        deps.discard(me)
        if eng == "pe" and not dma:
            deps = {d for d in deps if not (d[0] == "pe")}
        o = Op(fn, deps, dma)
        if dma:
            o.dmaid = me
            self.ndma[eng] += 1
        self.ops[eng].append(o)
        return me

    def emit(self):
        nc = self.nc
        for e in self.ENG:
            for o in self.ops[e]:
                for d in o.deps:
                    if d[0] != "dma":
                        self.ops[d[0]][d[1]].signal = True
        for e in self.ENG:
            for o in reversed(self.ops[e]):
                if not o.dma:
                    o.signal = True
                    break
        sigcount = {}
        for e in self.ENG:
            c = 0
            lst = []
            for o in self.ops[e]:
                if o.signal and not o.dma:
                    c += 1
                lst.append(c)
            sigcount[e] = lst
        G = Prog._g
        if G.get("nc") is not nc:
            G.clear()
            G["nc"] = nc
            G["s"] = {e: nc.alloc_semaphore(name=f"s_{e}") for e in self.ENG}
            G["sb"] = {e: 0 for e in self.ENG}
            G["bar"] = nc.alloc_semaphore(name="bar")
            G["barb"] = 0
            G["d"] = {}
            G["db"] = {}
        G["np"] = G.get("np", 0) + 1
        sems = {e: nc.alloc_semaphore(name=f"s{G['np']}_{e}") for e in self.ENG}
        sb = {e: 0 for e in self.ENG}
        bar = G["bar"]
        barb = G["barb"]
        NDS = self.NDS
        for e in self.ENG:
            if self.ndma[e] > 0 and e not in G["d"]:
                G["d"][e] = [nc.alloc_semaphore(name=f"d_{e}{i}") for i in range(NDS)]
                for i in range(NDS):
                    G["db"][(e, i)] = 0
        dsems = G["d"]
        db = dict(G["db"])

        def run(e, h):
            waited = {}
            waited_d = {}
            for i, o in enumerate(self.ops[e]):
                for d in sorted(o.deps, key=str):
                    if d[0] == "dma":
                        _, e2, k = d
                        s = dsems[e2][k % NDS]
                        v = db[(e2, k % NDS)] + 16 * (k // NDS + 1)
                        key = (e2, k % NDS)
                        if waited_d.get(key, 0) < v:
                            h.wait_ge(s, v)
                            waited_d[key] = v
                    else:
                        e2, j = d
                        c = sb[e2] + sigcount[e2][j]
                        if waited.get(e2, 0) < c:
                            h.wait_ge(sems[e2], c)
                            waited[e2] = c
                if o.dma:
                    k = o.dmaid[2]
                    s = dsems[e][k % NDS]
                    if k >= NDS:
                        v = db[(e, k % NDS)] + 16 * (k // NDS)
                        key = (e, k % NDS)
                        if waited_d.get(key, 0) < v:
                            h.wait_ge(s, v)
                            waited_d[key] = v
                    ins = o.fn(h)
                    ins.then_inc(s, 16)
                else:
                    ins = o.fn(h)
                    if o.signal:
                        ins.then_inc(sems[e], 1)
            if self.ndma[e] > 0:
                n = self.ndma[e]
                for q in range(min(NDS, n)):
                    cnt = (n - 1 - q) // NDS + 1
                    h.wait_ge(dsems[e][q], db[(e, q)] + 16 * cnt)
            if sigcount[e] and sigcount[e][-1] > 0:
                h.wait_ge(sems[e], sb[e] + sigcount[e][-1])
            h.sem_inc(bar, 1)
            h.wait_ge(bar, barb + 5)

        with nc.Block() as block:
            block.sync(lambda h: run("sp", h))
            block.tensor(lambda h: run("pe", h))
            block.scalar(lambda h: run("act", h))
            block.vector(lambda h: run("dve", h))
            block.gpsimd(lambda h: run("pool", h))
        for e in self.ENG:
            n = self.ndma[e]
            for q in range(min(NDS, n)):
                G["db"][(e, q)] += 16 * ((n - 1 - q) // NDS + 1)
        G["barb"] += 5

    def close(self):
        self.es.close()


class Buf:
    __slots__ = ("t", "k", "ps")

    def __init__(self, t, ps=False):
        self.t = t
        self.k = Tk()
        self.ps = ps

    def __getitem__(self, idx):
        return self.t[idx]


def _ks(bufs):
    return [b.k if isinstance(b, Buf) else b for b in bufs]


def _rw(reads, writes):
    r, w = [], []
    for b in reads:
        if isinstance(b, Buf) and b.ps:
            w.append(b.k)
        else:
            r.append(b.k if isinstance(b, Buf) else b)
    for b in writes:
        w.append(b.k if isinstance(b, Buf) else b)
    return r, w


class PX(Prog):
    def sbuf(self, shape, dtype, name=None):
        return Buf(self.sb(shape, dtype, name))

    def psum(self, shape, dtype=F32, name=None):
        return Buf(self.ps(shape, dtype, name), ps=True)

    def dma(self, q, out, in_, reads=(), writes=()):
        return self.op(q, lambda h: h.dma_start(out=out, in_=in_), *_rw(reads, writes), dma=True)

    def mm(self, out, pairs, reads=(), writes=()):
        n = len(pairs)

        def f(h):
            ins = None
            for i, (l, r) in enumerate(pairs):
                ins = h.matmul(out, lhsT=l, rhs=r, start=(i == 0), stop=(i == n - 1))
            return ins
        return self.op("pe", f, *_rw(reads, writes))

    def tr(self, out, in_, ident, reads=(), writes=()):
        return self.op("pe", lambda h: h.transpose(out=out, in_=in_, identity=ident), *_rw(reads, writes))

    def act(self, out, in_, func, reads=(), writes=(), **kw):
        return self.op("act", lambda h: h.activation(out=out, in_=in_, func=func, **kw), *_rw(reads, writes))

    def ts(self, eng, out, in0, s1, s2, op0, op1=None, reads=(), writes=(), **kw):
        if op1 is None:
            return self.op(eng, lambda h: h.tensor_scalar(out=out, in0=in0, scalar1=s1, scalar2=None, op0=op0, **kw),
                           *_rw(reads, writes))
        return self.op(eng, lambda h: h.tensor_scalar(out=out, in0=in0, scalar1=s1, scalar2=s2, op0=op0, op1=op1, **kw),
                       *_rw(reads, writes))

    def tt(self, eng, out, in0, in1, op, reads=(), writes=()):
        return self.op(eng, lambda h: h.tensor_tensor(out=out, in0=in0, in1=in1, op=op), *_rw(reads, writes))

    def stt(self, eng, out, in0, scalar, in1, op0, op1, reads=(), writes=()):
        return self.op(eng, lambda h: h.scalar_tensor_tensor(out=out, in0=in0, scalar=scalar, in1=in1, op0=op0, op1=op1),
                       *_rw(reads, writes))

    def copy(self, eng, out, in_, reads=(), writes=()):
        if eng == "act":
            return self.op("act", lambda h: h.copy(out=out, in_=in_), *_rw(reads, writes))
        return self.op(eng, lambda h: h.tensor_copy(out=out, in_=in_), *_rw(reads, writes))

    def memset(self, eng, ap, val, writes=()):
        return self.op(eng, lambda h: h.memset(ap, val), (), _ks(writes))


NT = 34
TP = 4358
EPS = 1e-6
NEGBIG = -30000.0


def pad_pos(tok):
    return tok + 2 if tok < 256 else tok + 4


def g1_inputs(nc):
    d = {}

    def inp(name, shape, dtype=F32):
        d[name] = nc.dram_tensor(name, list(shape), dtype, kind="ExternalInput").ap()
    inp("x", [4352, 1024])
    inp("cT", [128, 8, 2])
    inp("wmod", [1024, 2048])
    inp("bmod", [128, 16])
    inp("gmix", [128, 8])
    inp("wqkv", [1024, 1536])
    inp("wz", [1024, 512])
    inp("wab", [1024, 16])
    inp("convw", [128, 12, 5])
    inp("nalog", [128, 16])
    inp("dtb", [128, 16])
    inp("ogain", [128, 1])
    inp("cst", [128, 6 * 128])
    return d


def build_g1(nc, I, qkv_d, zT_d, gb_d):
    P = PX(nc)
    cst = P.sbuf([128, 6 * 128], F32)
    P.dma("sp", cst[:], I["cst"][:, :], writes=[cst])
    ident_f = cst[:, 0:128]
    ones_f = cst[:, 5 * 128:6 * 128]
    ident_b = P.sbuf([128, 128], BF16)
    P.copy("dve", ident_b[:], ident_f, reads=[cst], writes=[ident_b])

    cT = P.sbuf([128, 8, 2], F32)
    P.dma("sp", cT[:], I["cT"][:, :, :], writes=[cT])
    cact = P.sbuf([128, 8, 2], F32)
    P.act(cact[:], cT[:], AF.Silu, reads=[cT], writes=[cact])
    bmod = P.sbuf([128, 16], F32)
    P.dma("sp", bmod[:], I["bmod"][:, :], writes=[bmod])
    gmix = P.sbuf([128, 8], F32)
    P.dma("sp", gmix[:], I["gmix"][:, :], writes=[gmix])
    wm_ring = [P.sbuf([128, 8, 128], F32) for _ in range(3)]
    pp_ring = [P.psum([128, 512], F32) for _ in range(2)]
    pss_ring = [P.psum([128, 512], F32) for _ in range(2)]
    ps_mod = Buf(pp_ring[0][:, 0:32].rearrange("p (j s) -> p j s", s=2), ps=True)
    ps_mod.k = pp_ring[0].k
    wmod_v = I["wmod"].rearrange("(k p) c -> p k c", p=128)
    for j in range(16):
        wm = wm_ring[j % 3]
        P.dma("sp", wm[:], wmod_v[:, :, j * 128:(j + 1) * 128], writes=[wm])
        P.mm(ps_mod.t[:, j, :], [(wm[:, k, :], cact[:, k, :]) for k in range(8)], reads=[wm, cact], writes=[ps_mod])
    modc = P.sbuf([128, 16, 2], F32)
    for s in range(2):
        P.tt("dve", modc[:, :, s], ps_mod.t[:, :, s], bmod[:], ALU.add, reads=[ps_mod, bmod], writes=[modc])
    Acol = P.sbuf([128, 8, 2], F32)
    for s in range(2):
        P.stt("dve", Acol[:, :, s], modc[:, 8:16, s], 1.0, gmix[:], ALU.add, ALU.mult, reads=[modc, gmix], writes=[Acol])

    wqkv_b = P.sbuf([128, 8, 1536], BF16)
    wz_b = P.sbuf([128, 8, 512], BF16)
    wab_b = P.sbuf([128, 8, 16], BF16)
    stage = [P.sbuf([128, 2064], F32) for _ in range(2)]
    wq_v = I["wqkv"].rearrange("(k p) c -> p k c", p=128)
    wz_v = I["wz"].rearrange("(k p) c -> p k c", p=128)
    wab_v = I["wab"].rearrange("(k p) c -> p k c", p=128)
    for k in range(8):
        st = stage[k % 2]
        P.dma("sp", st[:, 0:1536], wq_v[:, k, :], writes=[st])
        P.dma("sp", st[:, 1536:2048], wz_v[:, k, :], writes=[st])
        P.dma("sp", st[:, 2048:2064], wab_v[:, k, :], writes=[st])
        P.copy("pool", wqkv_b[:, k, :], st[:, 0:1536], reads=[st], writes=[wqkv_b])
        P.copy("pool", wz_b[:, k, :], st[:, 1536:2048], reads=[st], writes=[wz_b])
        P.copy("pool", wab_b[:, k, :], st[:, 2048:2064], reads=[st], writes=[wab_b])
    convw = P.sbuf([128, 12, 5], F32)
    P.dma("sp", convw[:], I["convw"][:, :, :], writes=[convw])

    uT = P.sbuf([128, 8, TP], BF16)
    for (a, b) in ((0, 2), (258, 260), (4356, 4358)):
        P.memset("pool", uT[:, :, a:b], 0.0, writes=[uT])
    xt_ring = [P.sbuf([128, 1024], F32) for _ in range(3)]
    xn_ring = [P.sbuf([128, 1024], BF16) for _ in range(2)]
    junk = P.sbuf([128, 1024], BF16)
    ss_ring = [P.sbuf([128, 1], F32) for _ in range(3)]
    rs_ring = [P.sbuf([128, 1], F32) for _ in range(3)]
    ptr_ring = [P.psum([128, 8, 128], BF16) for _ in range(2)]
    for i in range(NT):
        xt = xt_ring[i % 3]
        xn = xn_ring[i % 2]
        ss = ss_ring[i % 3]
        rs = rs_ring[i % 3]
        ptr = ptr_ring[i % 2]
        P.dma("sp", xt[:], I["x"][i * 128:(i + 1) * 128, :], writes=[xt])
        P.act(junk[:], xt[:], AF.Square, reads=[xt], writes=[junk, ss], accum_out=ss[:])
        P.act(rs[:], ss[:], AF.Sqrt, reads=[ss], writes=[rs], scale=1.0 / 1024.0, bias=EPS)
        P.op("dve", (lambda h, rs=rs: h.reciprocal(out=rs[:], in_=rs[:])), _ks([rs]), _ks([rs]))
        P.act(xn[:], xt[:], AF.Copy, reads=[xt, rs], writes=[xn], scale=rs[:, 0:1])

        def trs(h, xn=xn, ptr=ptr):
            ins = None
            for k in range(8):
                ins = h.transpose(out=ptr[:, k, :], in_=xn[:, k * 128:(k + 1) * 128], identity=ident_b[:])
            return ins
        P.op("pe", trs, _ks([xn, ident_b]), _ks([ptr]))
        s = 1 if i < 2 else 0
        p0 = pad_pos(i * 128)
        for k in range(8):
            eng = "dve" if k % 2 == 0 else "act"
            if eng == "dve":
                P.ts("dve", uT[:, k, p0:p0 + 128], ptr[:, k, :], Acol[:, k, s:s + 1], modc[:, k, s:s + 1],
                     ALU.mult, ALU.add, reads=[ptr, Acol, modc], writes=[uT])
            else:
                P.act(uT[:, k, p0:p0 + 128], ptr[:, k, :], AF.Identity, reads=[ptr, Acol, modc], writes=[uT],
                      scale=Acol[:, k, s:s + 1], bias=modc[:, k, s:s + 1])

    nalog = P.sbuf([128, 16], F32)
    dtb = P.sbuf([128, 16], F32)
    P.dma("sp", nalog[:], I["nalog"][:, :], writes=[nalog])
    P.dma("sp", dtb[:], I["dtb"][:, :], writes=[dtb])
    nA = P.sbuf([128, 16], F32)
    P.act(nA[:], nalog[:], AF.Exp, reads=[nalog], writes=[nA])
    P.ts("dve", nA[:], nA[:], -1.0, None, ALU.mult, reads=[nA], writes=[nA])
    gb = P.sbuf([128, NT, 16], F32)
    ps_ab = []
    for r in pss_ring:
        bb = Buf(r[:, 0:272].rearrange("p (n c) -> p n c", c=16), ps=True)
        bb.k = r.k
        ps_ab.append(bb)
    for i in range(NT):
        p0 = pad_pos(i * 128)
        pab = ps_ab[i // 17]
        P.mm(pab.t[:, i % 17, :], [(uT[:, k, p0:p0 + 128], wab_b[:, k, :]) for k in range(8)],
             reads=[uT, wab_b], writes=[pab])
    tmp_ab = P.sbuf([128, NT, 16], F32)
    for hf in range(2):
        sl = slice(hf * 17, hf * 17 + 17)
        P.tt("dve", tmp_ab[:, sl, :], ps_ab[hf].t, dtb[:].unsqueeze(1).to_broadcast([128, 17, 16]), ALU.add,
             reads=[ps_ab[hf], dtb], writes=[tmp_ab])
    tv = tmp_ab[:].rearrange("p n (d w h) -> p n d w h", d=2, w=2)
    gv = gb[:].rearrange("p n (d w h) -> p n d w h", d=2, w=2)
    nAv = nA[:].rearrange("p (d w h) -> p d w h", d=2, w=2)
    e_ab = P.sbuf([128, NT, 16], F32)
    ev = e_ab[:].rearrange("p n (d w h) -> p n d w h", d=2, w=2)
    for d in range(2):
        P.act(ev[:, :, d, 0, :], tv[:, :, d, 0, :], AF.Exp, reads=[tmp_ab], writes=[e_ab])
    for d in range(2):
        P.act(ev[:, :, d, 0, :], ev[:, :, d, 0, :], AF.Ln, reads=[e_ab], writes=[e_ab], bias=1.0)
    for d in range(2):
        P.tt("dve", gv[:, :, d, 0, :], ev[:, :, d, 0, :], nAv[:, d, 0, :].unsqueeze(1).to_broadcast([128, NT, 4]),
             ALU.mult, reads=[e_ab, nA], writes=[gb])
    for d in range(2):
        P.act(gv[:, :, d, 1, :], tv[:, :, d, 1, :], AF.Sigmoid, reads=[tmp_ab], writes=[gb])
    P.dma("sp", gb_d.rearrange("n p c -> p n c"), gb[:], reads=[gb])

    NW = 9
    pb_ring = [P.sbuf([128, 512], F32) for _ in range(2)]
    aA_ring = [P.sbuf([128, 512], F32) for _ in range(2)]
    aB_ring = [P.sbuf([128, 512], F32) for _ in range(2)]
    s_ring = [P.sbuf([128, 512], F32) for _ in range(2)]
    sq_ring = [P.sbuf([128, 512], F32) for _ in range(2)]
    ri_ring = [P.sbuf([128, 512], F32) for _ in range(2)]
    ob_ring = [P.sbuf([128, 512], BF16) for _ in range(3)]
    it = 0
    items = [(j, w) for j in range(12) for w in range(NW)]

    def stageA(n):
        j, w = items[n]
        which = j % 3
        c0 = 508 * w
        L = min(512, TP - c0)
        Lo = L - 4
        pp = pp_ring[n % 2]
        pb, aA, aB, sb_, sq = (r[n % 2] for r in (pb_ring, aA_ring, aB_ring, s_ring, sq_ring))
        ob = ob_ring[n % 3]
        P.mm(pp[:, 0:L], [(wqkv_b[:, k, j * 128:(j + 1) * 128], uT[:, k, c0:c0 + L]) for k in range(8)],
             reads=[wqkv_b, uT], writes=[pp])
        P.act(aA[:, 0:Lo], pp[:, 0:Lo], AF.Copy, reads=[pp, convw], writes=[aA], scale=convw[:, j, 0:1])
        P.act(pb[:, 0:L], pp[:, 0:L], AF.Copy, reads=[pp], writes=[pb])
        P.stt("dve", aA[:, 0:Lo], pb[:, 1:1 + Lo], convw[:, j, 1:2], aA[:, 0:Lo], ALU.mult, ALU.add,
              reads=[pb, convw, aA], writes=[aA])
        P.stt("dve", aA[:, 0:Lo], pb[:, 2:2 + Lo], convw[:, j, 2:3], aA[:, 0:Lo], ALU.mult, ALU.add,
              reads=[pb, convw, aA], writes=[aA])
        P.act(aB[:, 0:Lo], pb[:, 3:3 + Lo], AF.Copy, reads=[pb, convw], writes=[aB], scale=convw[:, j, 3:4])
        P.stt("dve", aB[:, 0:Lo], pb[:, 4:4 + Lo], convw[:, j, 4:5], aB[:, 0:Lo], ALU.mult, ALU.add,
              reads=[pb, convw, aB], writes=[aB])
        P.tt("pool", aA[:, 0:Lo], aA[:, 0:Lo], aB[:, 0:Lo], ALU.add, reads=[aA, aB], writes=[aA])
        if which == 2:
            P.act(ob[:, 0:Lo], aA[:, 0:Lo], AF.Silu, reads=[aA], writes=[ob])
        else:
            P.act(sb_[:, 0:Lo], aA[:, 0:Lo], AF.Silu, reads=[aA], writes=[sb_])
            P.tt("pool", sq[:, 0:Lo], sb_[:, 0:Lo], sb_[:, 0:Lo], ALU.mult, reads=[sb_], writes=[sq])

    def stageB(n):
        j, w = items[n]
        hl, which = j // 3, j % 3
        c0 = 508 * w
        L = min(512, TP - c0)
        Lo = L - 4
        pss = pss_ring[n % 2]
        sb_, sq, ri = (r[n % 2] for r in (s_ring, sq_ring, ri_ring))
        ob = ob_ring[n % 3]
        if which != 2:
            P.mm(pss[:, 0:Lo], [(ones_f, sq[:, 0:Lo])], reads=[cst, sq], writes=[pss])
            P.act(ri[:, 0:Lo], pss[:, 0:Lo], AF.Sqrt, reads=[pss], writes=[ri], bias=EPS)
            P.op("dve", (lambda h, ri=ri, Lo=Lo: h.reciprocal(out=ri[:, 0:Lo], in_=ri[:, 0:Lo])), _ks([ri]), _ks([ri]))
            sc = (128.0 ** -0.5) if which == 0 else 1.0
            P.stt("dve", ob[:, 0:Lo], sb_[:, 0:Lo], sc, ri[:, 0:Lo], ALU.mult, ALU.mult, reads=[sb_, ri], writes=[ob])
        dst = qkv_d[hl, which]
        if w == 0:
            P.dma("sp", dst[:, 0:256], ob[:, 0:256], reads=[ob])
            P.dma("sp", dst[:, 256:256 + (Lo - 258)], ob[:, 258:Lo], reads=[ob])
        else:
            t0 = c0 + 2 - 4
            P.dma("sp", dst[:, t0:t0 + Lo], ob[:, 0:Lo], reads=[ob])

    for n in range(len(items) + 1):
        if n < len(items):
            stageA(n)
        if n >= 1:
            stageB(n - 1)
    it = len(items)

    for hl in range(4):
        for gq in range(8):
            pp = pp_ring[it % 2]
            ob = ob_ring[it % 3]
            it += 1
            c0 = 260 + 512 * gq
            P.mm(pp[:], [(wz_b[:, k, hl * 128:(hl + 1) * 128], uT[:, k, c0:c0 + 512]) for k in range(8)],
                 reads=[wz_b, uT], writes=[pp])
            P.act(ob[:], pp[:], AF.Silu, reads=[pp], writes=[ob])
            P.dma("sp", zT_d[hl][:, gq * 512:(gq + 1) * 512], ob[:], reads=[ob])
    P.emit()
    P.close()


def g1_host_inputs(inp, core):
    b, g = core // 2, core % 2
    heads = [4 * g + i for i in range(4)]
    d = {}
    d["x"] = np.ascontiguousarray(np.concatenate([inp["ctx"][b], inp["x"][b]], axis=0))
    cT = np.zeros((128, 8, 2), np.float32)
    cT[:, :, 0] = inp["c"][b].reshape(8, 128).T
    cT[:, :, 1] = inp["c_ctx"].reshape(8, 128).T
    d["cT"] = cT
    d["wmod"] = np.ascontiguousarray(inp["w_mod"][0][:, 0:2048])
    d["bmod"] = np.ascontiguousarray(inp["b_mod"][0][0:2048].reshape(16, 128).T)
    d["gmix"] = np.ascontiguousarray(inp["norm_mix_g"][0].reshape(8, 128).T)
    w_in = inp["gdn_w_in"][0]
    cols = []
    for hl, h in enumerate(heads):
        for which in range(3):
            cols.append(np.arange(which * 1024 + h * 128, which * 1024 + (h + 1) * 128))
    cols = np.concatenate(cols)
    d["wqkv"] = np.ascontiguousarray(w_in[:, cols])
    zc = np.concatenate([np.arange(3072 + h * 128, 3072 + (h + 1) * 128) for h in heads])
    d["wz"] = np.ascontiguousarray(w_in[:, zc])
    abc = np.array([4096 + dd * 16 + w * 8 + h for dd in range(2) for w in range(2) for h in heads])
    d["wab"] = np.ascontiguousarray(w_in[:, abc])
    cw = inp["gdn_conv_w"][0][:, cols]
    d["convw"] = np.ascontiguousarray(cw.reshape(5, 12, 128).transpose(2, 1, 0))
    nalog = np.zeros((128, 16), np.float32)
    dtb = np.zeros((128, 16), np.float32)
    for dd in range(2):
        for hl, h in enumerate(heads):
            nalog[:, dd * 8 + hl] = inp["gdn_a_log"][0][dd, h]
            dtb[:, dd * 8 + hl] = inp["gdn_dt_bias"][0][dd, h]
    d["nalog"] = nalog
    d["dtb"] = dtb
    d["ogain"] = np.ascontiguousarray(inp["gdn_o_gain"][0].reshape(128, 1))
    d["cst"] = make_cst()
    return d


def make_cst():
    i = np.arange(128)
    ident = np.eye(128, dtype=np.float32)
    tri0 = (i[:, None] <= i[None, :]).astype(np.float32)
    tri1 = (i[:, None] >= i[None, :]).astype(np.float32)
    negS0 = np.where(i[None, :] > i[:, None], 0.0, NEGBIG).astype(np.float32)
    negS1 = np.where(i[None, :] < i[:, None], 0.0, NEGBIG).astype(np.float32)
    ones = np.ones((128, 128), np.float32)
    return np.ascontiguousarray(np.concatenate([ident, tri0, tri1, negS0, negS1, ones], axis=1))


def tile_of(d, s):
    if d == 0:
        return s
    return (1 - s) if s < 2 else 35 - s


def build_g2(nc, I, qkv_d, zT_d, gb_d, ogT_out, nsteps=34, ngate=4):
    P = PX(nc)
    cst = P.sbuf([128, 6 * 128], F32)
    P.dma("sp", cst[:], I["cst"][:, :], writes=[cst])
    ident_f = cst[:, 0:128]
    tri = [cst[:, 128:256], cst[:, 256:384]]
    negS = [cst[:, 384:512], cst[:, 512:640]]
    ones_f = cst[:, 640:768]
    ident_b = P.sbuf([128, 128], BF16)
    P.copy("dve", ident_b[:], ident_f, reads=[cst], writes=[ident_b])
    ogain = P.sbuf([128, 1], F32)
    P.dma("sp", ogain[:], I["ogain"][:, :], writes=[ogain])
    gb = P.sbuf([128, NT, 16], F32)
    P.dma("sp", gb[:], gb_d.rearrange("n p c -> p n c"), writes=[gb])
    oacc = P.sb([128, 4, 4096], F32)
    oacc_k = [[Tk() for _ in range(32)] for _ in range(4)]
    for hl in range(4):
        P.op("pool", (lambda h, hl=hl: h.memset(oacc[:, hl, :], 0.0)), (), oacc_k[hl])
    chains = [(hl, d) for hl in range(4) for d in range(2)]
    Sf = {c: [P.sbuf([128, 128], F32) for _ in range(2)] for c in chains}
    Sb = {c: [P.sbuf([128, 128], BF16) for _ in range(2)] for c in chains}
    for c in chains:
        P.memset("pool", Sf[c][0][:], 0.0, writes=[Sf[c][0]])
        P.memset("pool", Sb[c][0][:], 0.0, writes=[Sb[c][0]])
    prod = {c: [dict(P=P.sbuf([128, 128], BF16), WnT=P.sbuf([128, 128], BF16), qdT=P.sbuf([128, 128], BF16),
                     attT=P.sbuf([128, 128], BF16), kt=P.sbuf([128, 128], BF16), v=P.sbuf([128, 128], BF16),
                     glast=P.sbuf([128, 1], F32), vnew=P.sbuf([128, 128], BF16)) for _ in range(2)] for c in chains}

    class Ring:
        def __init__(self, mk, n):
            self.b = [mk() for _ in range(n)]
            self.i = 0

        def get(self):
            x = self.b[self.i % len(self.b)]
            self.i += 1
            return x
    def carve(bank, n):
        out = []
        for i in range(n):
            bb = Buf(bank.t[:, i * 128:(i + 1) * 128], ps=True)
            bb.k = bank.k
            out.append(bb)
        return out
    _pb0 = carve(P.psum([128, 1024], BF16), 8)
    _pb1 = carve(P.psum([128, 1024], BF16), 8)
    _pb = [x for pair in zip(_pb0, _pb1) for x in pair]
    _banks = [carve(P.psum([128, 512], F32), 4) for _ in range(3)]
    _pf = [_banks[b][i] for i in range(4) for b in range(3)]
    _p2 = []
    for _ in range(2):
        bk = P.psum([128, 512], F32)
        for i in range(2):
            bb = Buf(bk.t[:, i * 256:(i + 1) * 256], ps=True)
            bb.k = bk.k
            _p2.append(bb)
    _p2 = [_p2[0], _p2[2], _p2[1], _p2[3]]
    psf2 = Ring(None, 0)
    psf2.b = _p2
    psb = Ring(None, 0)
    psb.b = _pb
    psf = Ring(None, 0)
    psf.b = _pf
    pss = P.psum([128, 512], F32)
    tmp = {}
    for c in chains:
        t = {}
        t["qkv"] = [P.sbuf([128, 3, 128], BF16) for _ in range(2)]
        for nm in ("TriG", "X", "DS", "E", "DI"):
            t[nm] = P.sbuf([128, 128], F32)
        for nm in ("ktm", "Ke", "N", "NTt", "IpNT", "Rm", "PT"):
            t[nm] = P.sbuf([128, 128], BF16)
        t["XP"] = [P.sbuf([128, 256], BF16) for _ in range(2)]
        t["XTc"] = [P.sbuf([128, 128], BF16) for _ in range(2)]
        for nm in ("ngc", "egc", "lastc", "ktc"):
            t[nm] = P.sbuf([128, 1], F32)
        tmp[c] = t
    evq = [0]

    def evac(out, in_, reads, writes):
        evq[0] += 1
        P.copy("act" if evq[0] % 2 else "dve", out, in_, reads=reads, writes=writes)

    def precompute(stage, c, s, st):
        hl, d = c
        tl = tile_of(d, s)
        pr = prod[c][s % 2]
        t = tmp[c]
        qkv = t["qkv"][s % 2]
        gcol = gb[:, tl, d * 8 + hl:d * 8 + hl + 1]
        bcol = gb[:, tl, d * 8 + 4 + hl:d * 8 + 4 + hl + 1]
        if stage == 0:
            P.dma("sp", qkv[:], qkv_d[hl][:, :, tl * 128:(tl + 1) * 128].rearrange("w p t -> p w t"), writes=[qkv])
            P.act(t["TriG"][:], tri[d], AF.Copy, reads=[cst, gb], writes=[t["TriG"]], scale=gcol)
        elif stage == 1:
            pk, pv = psb.get(), psb.get()
            P.tr(pk[:], qkv[:, 1, :], ident_b[:], reads=[qkv, ident_b], writes=[pk])
            P.tr(pv[:], qkv[:, 2, :], ident_b[:], reads=[qkv, ident_b], writes=[pv])
            evac(t["ktm"][:], pk[:], [pk], [t["ktm"]])
            evac(pr["v"][:], pv[:], [pv], [pr["v"]])
            KK, KQ, EB, gcc = psf.get(), psf.get(), psf.get(), psf.get()
            P.mm(KK[:], [(qkv[:, 1, :], qkv[:, 1, :])], reads=[qkv], writes=[KK])
            P.mm(KQ[:], [(qkv[:, 1, :], qkv[:, 0, :])], reads=[qkv], writes=[KQ])
            P.mm(EB[:], [(ones_f, t["TriG"][:])], reads=[cst, t["TriG"]], writes=[EB])
            P.mm(gcc[:, 0:1], [(tri[d], gcol)], reads=[cst, gb], writes=[gcc])
            last = 127 if d == 0 else 0
            ngc, egc, lastc, ktc = t["ngc"], t["egc"], t["lastc"], t["ktc"]
            P.act(ngc[:], gcc[:, 0:1], AF.Copy, reads=[gcc], writes=[ngc], scale=-1.0)
            P.act(lastc[:], EB[:, last:last + 1], AF.Copy, reads=[EB], writes=[lastc])
            X, DS, E, DI = t["X"], t["DS"], t["E"], t["DI"]
            P.tt("dve", X[:], EB[:], negS[d], ALU.add, reads=[EB, cst], writes=[X])
            P.act(DS[:], X[:], AF.Exp, reads=[X, ngc], writes=[DS], bias=ngc[:, 0:1])
            P.act(E[:], EB[:], AF.Exp, reads=[EB], writes=[E])
            P.act(egc[:], gcc[:, 0:1], AF.Exp, reads=[gcc], writes=[egc])
            P.act(pr["glast"][:], lastc[:], AF.Exp, reads=[lastc], writes=[pr["glast"]])
            P.act(ktc[:], gcc[:, 0:1], AF.Exp, reads=[gcc, lastc], writes=[ktc], scale=-1.0, bias=lastc[:, 0:1])
            P.tt("pool", DI[:], DS[:], ident_f, ALU.add, reads=[DS, cst], writes=[DI])
            N = t["N"]
            P.stt("dve", N[:], KK[:], bcol, DS[:], ALU.mult, ALU.mult, reads=[KK, gb, DS], writes=[N])
            P.tt("dve", pr["attT"][:], KQ[:], DI[:], ALU.mult, reads=[KQ, DI], writes=[pr["attT"]])
            P.tt("pool", pr["qdT"][:], qkv[:, 0, :], E[:], ALU.mult, reads=[qkv, E], writes=[pr["qdT"]])
            P.act(t["Ke"][:], t["ktm"][:], AF.Copy, reads=[t["ktm"], egc], writes=[t["Ke"]], scale=egc[:, 0:1])
            P.act(pr["kt"][:], t["ktm"][:], AF.Copy, reads=[t["ktm"], ktc], writes=[pr["kt"]], scale=ktc[:, 0:1])
            P.tt("pool", t["XP"][1][:, 128:256], ident_b[:], N[:], ALU.subtract, reads=[ident_b, N], writes=[t["XP"][1]])
        elif stage == 2:
            N = t["N"]
            pn = psb.get()
            P.tr(pn[:], N[:], ident_b[:], reads=[N, ident_b], writes=[pn])
            evac(t["NTt"][:], pn[:], [pn], [t["NTt"]])
            P.tt("pool", t["IpNT"][:], t["NTt"][:], ident_b[:], ALU.add, reads=[t["NTt"], ident_b], writes=[t["IpNT"]])
        elif stage == 3:
            N, NTt = t["N"], t["NTt"]
            p1 = psf.get()
            P.mm(p1[:], [(NTt[:], N[:])], reads=[N, NTt], writes=[p1])
            evac(t["XP"][1][:, 0:128], p1[:], [p1], [t["XP"][1]])
            p2 = psf.get()
            P.mm(p2[:], [(N[:], NTt[:])], reads=[N, NTt], writes=[p2])
            evac(t["XTc"][1][:], p2[:], [p2], [t["XTc"][1]])
        elif 4 <= stage <= 9:
            lev = stage - 3
            XP, XT = t["XP"][lev % 2], t["XTc"][lev % 2]
            XPn, XTn = t["XP"][(lev + 1) % 2], t["XTc"][(lev + 1) % 2]
            pa2 = psf2.get()
            if lev < 6:
                P.mm(pa2[:], [(XT[:], XP[:])], reads=[XT, XP], writes=[pa2])
                evac(XPn[:, 0:128], pa2[:, 0:128], [pa2], [XPn])
                P.tt("dve", XPn[:, 128:256], pa2[:, 128:256], XP[:, 128:256], ALU.add, reads=[pa2, XP], writes=[XPn])
                p2 = psf.get()
                P.mm(p2[:], [(XP[:, 0:128], XT[:])], reads=[XP, XT], writes=[p2])
                evac(XTn[:], p2[:], [p2], [XTn])
            else:
                P.mm(pa2[:, 0:128], [(XT[:], XP[:, 128:256])], reads=[XT, XP], writes=[pa2])
                P.tt("dve", XPn[:, 128:256], pa2[:, 0:128], XP[:, 128:256], ALU.add, reads=[pa2, XP], writes=[XPn])
        elif stage == 10:
            Pt = t["XP"][1][:, 128:256]
            Ptk = t["XP"][1]
            pr_ = psf.get()
            P.mm(pr_[:], [(t["IpNT"][:], Pt)], reads=[t["IpNT"], Ptk], writes=[pr_])
            P.tt("dve", t["Rm"][:], ident_f, pr_[:], ALU.subtract, reads=[cst, pr_], writes=[t["Rm"]])
            ptp = psb.get()
            P.tr(ptp[:], Pt, ident_b[:], reads=[Ptk, ident_b], writes=[ptp])
            evac(t["PT"][:], ptp[:], [ptp], [t["PT"]])
        elif stage == 11:
            Pt = t["XP"][1][:, 128:256]
            Ptk = t["XP"][1]
            pc_ = psf.get()
            P.mm(pc_[:], [(t["PT"][:], t["Rm"][:])], reads=[t["PT"], t["Rm"]], writes=[pc_])
            P.tt("dve", pr["P"][:], pc_[:], Pt, ALU.add, reads=[pc_, Ptk], writes=[pr["P"]])
        elif stage == 12:
            pw = psf.get()
            P.mm(pw[:], [(t["Ke"][:], pr["P"][:])], reads=[t["Ke"], pr["P"]], writes=[pw])
            P.act(pr["WnT"][:], pw[:], AF.Copy, reads=[pw], writes=[pr["WnT"]], scale=-1.0)

    NSTAGE = 13

    def scan(sub, c, s, st):
        hl, d = c
        tl = tile_of(d, s)
        pr = prod[c][s % 2]
        So_f, Sn_f = Sf[c][s % 2], Sf[c][(s + 1) % 2]
        So_b, Sn_b = Sb[c][s % 2], Sb[c][(s + 1) % 2]
        if sub == 0:
            pa = psf.get()
            P.mm(pa[:], [(pr["P"][:], pr["v"][:]), (pr["WnT"][:], So_b[:])], reads=[pr["P"], pr["v"], pr["WnT"], So_b], writes=[pa])
            st["pa"] = pa
        elif sub == 1:
            bcol = gb[:, tl, d * 8 + 4 + hl:d * 8 + 4 + hl + 1]
            P.act(pr["vnew"][:], st["pa"][:], AF.Copy, reads=[st["pa"], gb], writes=[pr["vnew"]], scale=bcol)
        elif sub == 2:
            pso = psf.get()
            if tl >= 2:
                P.mm(pso[:], [(So_b[:], pr["qdT"][:]), (pr["vnew"][:], pr["attT"][:])],
                     reads=[So_b, pr["qdT"], pr["vnew"], pr["attT"]], writes=[pso])
            ps_ = psf.get()
            P.mm(ps_[:], [(pr["kt"][:], pr["vnew"][:])], reads=[pr["kt"], pr["vnew"]], writes=[ps_])
            st["pso"], st["ps"] = pso, ps_
        elif sub == 3:
            P.stt("dve", Sn_b[:], So_f[:], pr["glast"][:, 0:1], st["ps"][:], ALU.mult, ALU.add,
                  reads=[So_f, pr["glast"], st["ps"]], writes=[Sn_b])
            P.stt("dve", Sn_f[:], So_f[:], pr["glast"][:, 0:1], st["ps"][:], ALU.mult, ALU.add,
                  reads=[So_f, pr["glast"], st["ps"]], writes=[Sn_f])
            if tl >= 2:
                n = tl - 2
                osl = oacc[:, hl, n * 128:(n + 1) * 128]
                P.op("dve", (lambda h, osl=osl, pso=st["pso"]: h.tensor_tensor(out=osl, in0=pso[:], in1=osl, op=ALU.add)),
                     [], _ks([st["pso"], oacc_k[hl][n]]))

    pst = {}
    sst = {}
    for s in range(nsteps + 1):
        if s < nsteps:
            for c in chains:
                pst[c] = {}
            for stage in range(NSTAGE):
                for c in chains:
                    precompute(stage, c, s, pst[c])
        if s >= 1:
            for c in chains:
                sst[c] = {}
            for sub in range(2):
                for c in chains:
                    scan(sub, c, s - 1, sst[c])
            for half in range(2):
                grp = chains[half * 4:(half + 1) * 4]
                for sub in (2, 3):
                    for c in grp:
                        scan(sub, c, s - 1, sst[c])

    g_sq = [P.sbuf([128, 512], F32) for _ in range(2)]
    g_rs = [P.sbuf([128, 512], F32) for _ in range(2)]
    g_z = [P.sbuf([128, 512], BF16) for _ in range(2)]
    g_o = [P.sbuf([128, 512], BF16) for _ in range(2)]
    it = 0
    for hl in range(ngate):
        for gq in range(8):
            sq, rs, zt, og = g_sq[it % 2], g_rs[it % 2], g_z[it % 2], g_o[it % 2]
            it += 1
            oks = oacc_k[hl][gq * 4:(gq + 1) * 4]
            osl = oacc[:, hl, gq * 512:(gq + 1) * 512]
            P.dma("sp", zt[:], zT_d[hl][:, gq * 512:(gq + 1) * 512], writes=[zt])
            P.op("act", (lambda h, sq=sq, osl=osl: h.activation(out=sq[:], in_=osl, func=AF.Square)), _ks(oks), _ks([sq]))
            P.mm(pss[:], [(ones_f, sq[:])], reads=[cst, sq], writes=[pss])
            P.act(rs[:], pss[:], AF.Sqrt, reads=[pss], writes=[rs], scale=1.0 / 128.0, bias=EPS)
            P.op("dve", (lambda h, rs=rs: h.reciprocal(out=rs[:], in_=rs[:])), _ks([rs]), _ks([rs]))
            P.op("dve", (lambda h, rs=rs, osl=osl: h.tensor_tensor(out=rs[:], in0=osl, in1=rs[:], op=ALU.mult)),
                 _ks(oks + [rs]), _ks([rs]))
            P.stt("dve", og[:], rs[:], ogain[:, 0:1], zt[:], ALU.mult, ALU.mult, reads=[rs, ogain, zt], writes=[og])
            P.dma("sp", ogT_out[hl * 128:(hl + 1) * 128, gq * 512:(gq + 1) * 512], og[:], reads=[og])
    P.emit()
    P.close()


NTO = 32


def make_cst2():
    i = np.arange(128)
    ident = np.eye(128, dtype=np.float32)
    tristrict = (i[:, None] < i[None, :]).astype(np.float32)
    ones = np.ones((128, 128), np.float32)
    erow = np.tile((np.arange(16) * 512).astype(np.float32)[None, :], (128, 1))
    pad = np.zeros((128, 112), np.float32)
    return np.ascontiguousarray(np.concatenate([ident, tristrict, ones, erow, pad], axis=1))


def mod_rows(P, nc, cact, ones_f, wmod_ap, bmod_ap, col0, ncols, pbank, out_bc, stage):
    wv = wmod_ap.rearrange("(k p) c -> p k c", p=128)
    P.dma("sp", out_bc[:, 0:ncols], bmod_ap[col0:col0 + ncols].partition_broadcast(128), writes=[out_bc])
    for hh in range(ncols // 512):
        for k in range(8):
            P.dma("sp", stage[:, k, :], wv[:, k, col0 + hh * 512:col0 + (hh + 1) * 512], writes=[stage])
        pb = pbank[hh % len(pbank)]
        P.mm(pb[:], [(cact[:, k, :], stage[:, k, :]) for k in range(8)], reads=[cact, stage], writes=[pb])
        P.tt("dve", out_bc[:, hh * 512:(hh + 1) * 512], pb[:], out_bc[:, hh * 512:(hh + 1) * 512], ALU.add,
             reads=[pb, out_bc], writes=[out_bc])


def load_cact(P, I, cst):
    ones_f = cst[:, 256:384]
    cT = P.sbuf([128, 8], F32)
    P.dma("sp", cT[:], I["cT"][:, :], writes=[cT])
    ca = P.sbuf([128, 8], F32)
    P.act(ca[:], cT[:], AF.Silu, reads=[cT], writes=[ca])
    cactB = P.sbuf([128, 8, 128], F32)
    for k in range(8):
        P.ts("dve", cactB[:, k, :], ones_f, ca[:, k:k + 1], None, ALU.mult, reads=[cst, ca], writes=[cactB])
    return cactB


def c0_inputs(nc):
    d = {}

    def inp(name, shape, dtype=F32):
        d[name] = nc.dram_tensor(name, list(shape), dtype, kind="ExternalInput").ap()
    inp("ogT", [1024, 2048], BF16)
    inp("xo", [2048, 1024])
    inp("cT", [128, 8])
    inp("wmod", [1024, 6144])
    inp("bmod", [6144])
    inp("gffn", [1024])
    inp("wout", [1024, 1024])
    inp("wr", [1024, 16])
    inp("cst2", [128, 512])
    return d


def norm_router(P, I, cst, ident_f, h_tile, A2, B2, wr, rings, aff_sb, u2_out_ap, i):
    junk, ss, rs, t2, u2b, u2T, pT, pl, mx, sm = rings
    P.act(junk[:], h_tile[:], AF.Square, reads=[h_tile], writes=[junk, ss], accum_out=ss[:])
    P.act(rs[:], ss[:], AF.Sqrt, reads=[ss], writes=[rs], scale=1.0 / 1024.0, bias=EPS)
    P.op("dve", (lambda h, rs=rs: h.reciprocal(out=rs[:], in_=rs[:])), _ks([rs]), _ks([rs]))
    P.stt("dve", t2[:], h_tile[:], rs[:, 0:1], A2[:], ALU.mult, ALU.mult, reads=[h_tile, rs, A2], writes=[t2])
    P.tt("pool", t2[:], t2[:], B2[:], ALU.add, reads=[t2, B2], writes=[t2])
    P.copy("act", u2b[:], t2[:], reads=[t2], writes=[u2b])
    P.dma("sp", u2_out_ap, u2b[:], reads=[u2b])
    for hh in range(2):
        def trs(h, hh=hh, t2=t2, pT=pT):
            ins = None
            for k in range(4):
                kk = hh * 4 + k
                ins = h.transpose(out=pT[hh][:, k * 128:(k + 1) * 128], in_=t2[:, kk * 128:(kk + 1) * 128], identity=ident_f)
            return ins
        P.op("pe", trs, _ks([t2, cst]), _ks([pT[hh]]))
        P.copy("act" if hh == 0 else "dve", u2T[:, hh * 512:(hh + 1) * 512], pT[hh][:], reads=[pT[hh]], writes=[u2T])
    P.mm(pl[:, 0:16], [(u2T[:, k * 128:(k + 1) * 128], wr[:, k, :]) for k in range(8)], reads=[u2T, wr], writes=[pl])
    P.op("dve", (lambda h, mx=mx, pl=pl: h.reduce_max(out=mx[:], in_=pl[:, 0:16], axis=AX.X)), [], _ks([pl, mx]))
    P.ts("dve", mx[:], mx[:], -1.0, None, ALU.mult, reads=[mx], writes=[mx])
    P.act(aff_sb[:, i, :], pl[:, 0:16], AF.Exp, reads=[pl, mx], writes=[aff_sb, sm], bias=mx[:, 0:1], accum_out=sm[:])
    P.op("dve", (lambda h, sm=sm: h.reciprocal(out=sm[:], in_=sm[:])), _ks([sm]), _ks([sm]))
    P.ts("dve", aff_sb[:, i, :], aff_sb[:, i, :], sm[:, 0:1], None, ALU.mult, reads=[aff_sb, sm], writes=[aff_sb])


def make_nr_rings(P):
    junk = P.sbuf([128, 1024], BF16)
    ss = P.sbuf([128, 1], F32)
    rs = P.sbuf([128, 1], F32)
    t2 = P.sbuf([128, 1024], F32)
    u2b = P.sbuf([128, 1024], BF16)
    u2T = P.sbuf([128, 1024], F32)
    pT = [P.psum([128, 512], F32) for _ in range(2)]
    pl = P.psum([128, 512], F32)
    mx = P.sbuf([128, 1], F32)
    sm = P.sbuf([128, 1], F32)
    return (junk, ss, rs, t2, u2b, u2T, pT, pl, mx, sm)


def ffn_mod_setup(P, nc, I, cst, cactB, layer, pbank, stage):
    ones_f = cst[:, 256:384]
    base = 0
    B2 = P.sbuf([128, 1024], F32)
    A2 = P.sbuf([128, 1024], F32)
    mod_rows(P, nc, cactB, ones_f, I["wmod"], I["bmod"], 3072, 1024, pbank, B2, stage)
    mod_rows(P, nc, cactB, ones_f, I["wmod"], I["bmod"], 4096, 1024, pbank, A2, stage)
    gf = P.sbuf([128, 1024], F32)
    P.dma("sp", gf[:], I["gffn"].partition_broadcast(128), writes=[gf])
    P.stt("dve", A2[:], A2[:], 1.0, gf[:], ALU.add, ALU.mult, reads=[A2, gf], writes=[A2])
    return A2, B2


def build_c0(nc, I, ogT_ap, x_ap, h1_out, u2_out, aff_out):
    P = PX(nc)
    cst = P.sbuf([128, 512], F32)
    P.dma("sp", cst[:], I["cst2"][:, :], writes=[cst])
    ident_f = cst[:, 0:128]
    cactB = load_cact(P, I, cst)
    pbank = [P.psum([128, 512], F32) for _ in range(2)]
    stage = P.sbuf([128, 8, 512], F32)
    gtm = P.sbuf([128, 1024], F32)
    mod_rows(P, nc, cactB, cst[:, 256:384], I["wmod"], I["bmod"], 2048, 1024, pbank, gtm, stage)
    A2, B2 = ffn_mod_setup(P, nc, I, cst, cactB, 0, pbank, stage)
    wout_b = P.sbuf([128, 8, 1024], BF16)
    wv = I["wout"].rearrange("(k p) c -> p k c", p=128)
    for hh in range(2):
        for k in range(8):
            P.dma("sp", stage[:, k, :], wv[:, k, hh * 512:(hh + 1) * 512], writes=[stage])
        for k in range(8):
            P.copy("pool", wout_b[:, k, hh * 512:(hh + 1) * 512], stage[:, k, :], reads=[stage], writes=[wout_b])
    wr = P.sbuf([128, 8, 16], F32)
    P.dma("sp", wr[:], I["wr"].rearrange("(k p) c -> p k c", p=128), writes=[wr])
    aff_sb = P.sbuf([128, NTO, 16], F32)
    rings = make_nr_rings(P)
    og_ring = [P.sbuf([128, 8, 128], BF16) for _ in range(2)]
    x_ring = [P.sbuf([128, 1024], F32) for _ in range(2)]
    h_ring = [P.sbuf([128, 1024], F32) for _ in range(2)]
    ogv = ogT_ap.rearrange("(k p) t -> p k t", p=128)
    for i in range(NTO):
        og, xt, ht = og_ring[i % 2], x_ring[i % 2], h_ring[i % 2]
        P.dma("sp", og[:], ogv[:, :, i * 128:(i + 1) * 128], writes=[og])
        P.dma("sp", xt[:], x_ap[i * 128:(i + 1) * 128, :], writes=[xt])
        for hh in range(2):
            pb = pbank[hh]
            P.mm(pb[:], [(og[:, k, :], wout_b[:, k, hh * 512:(hh + 1) * 512]) for k in range(8)], reads=[og, wout_b], writes=[pb])
            P.tt("dve", ht[:, hh * 512:(hh + 1) * 512], pb[:], gtm[:, hh * 512:(hh + 1) * 512], ALU.mult,
                 reads=[pb, gtm], writes=[ht])
        P.tt("pool", ht[:], ht[:], xt[:], ALU.add, reads=[ht, xt], writes=[ht])
        P.dma("sp", h1_out[i * 128:(i + 1) * 128, :], ht[:], reads=[ht])
        norm_router(P, I, cst, ident_f, ht, A2, B2, wr, rings, aff_sb, u2_out[i * 128:(i + 1) * 128, :], i)
    P.dma("sp", aff_out.rearrange("(n p) e -> p n e", p=128), aff_sb[:], reads=[aff_sb])
    P.emit()
    P.close()


BIGIDX = 1048576.0


def r_inputs(nc):
    d = {}

    def inp(name, shape, dtype=F32):
        d[name] = nc.dram_tensor(name, list(shape), dtype, kind="ExternalInput").ap()
    inp("affB", [4096, 16])
    inp("affO", [2048, 16])
    inp("u2o", [2048, 1024], BF16)
    inp("slotab", [128, 2])
    inp("cst2", [128, 512])
    return d


def build_r(nc, I, aff_ap, u2_ap, xs_part, idx_out, gate_out):
    P = PX(nc)
    cst = P.sbuf([128, 512], F32)
    P.dma("sp", cst[:], I["cst2"][:, :], writes=[cst])
    ones_b = P.sbuf([128, 128], BF16)
    tri_b = P.sbuf([128, 128], BF16)
    P.copy("dve", ones_b[:], cst[:, 256:384], reads=[cst], writes=[ones_b])
    P.copy("dve", tri_b[:], cst[:, 128:256], reads=[cst], writes=[tri_b])
    erow = cst[:, 384:400]
    affB = P.sbuf([128, 32, 16], F32)
    P.dma("sp", affB[:], aff_ap.rearrange("(n p) e -> p n e", p=128), writes=[affB])
    affO = affB
    u2s = P.sbuf([128, NTO, 1024], BF16)
    for j in range(NTO):
        P.dma("sp", u2s[:, j, :], u2_ap[j * 128:(j + 1) * 128, :], writes=[u2s])
    zks = []
    lo = P.sbuf([128, 16], F32)
    mid = P.sbuf([128, 16], F32)
    cnt = P.sbuf([128, 16], F32)
    inc = P.sbuf([128, 16], F32)
    maskb = P.sbuf([128, 32, 16], BF16)
    pc = P.psum([128, 512], F32)
    P.memset("dve", lo[:], 0.0, writes=[lo])
    for it in range(30):
        step = 2.0 ** -(it + 1)
        P.ts("dve", mid[:], lo[:], step, None, ALU.add, reads=[lo], writes=[mid])
        P.tt("dve", maskb[:], affB[:], mid[:].unsqueeze(1).to_broadcast([128, 32, 16]), ALU.is_ge,
             reads=[affB, mid], writes=[maskb])
        P.mm(pc[:], [(ones_b[:], maskb[:].rearrange("p n e -> p (n e)"))], reads=[ones_b, maskb], writes=[pc])
        P.op("dve", (lambda h: h.tensor_reduce(out=cnt[:], in_=pc[:].rearrange("p (n e) -> p e n", e=16),
                                                axis=AX.X, op=ALU.add)), [], _ks([pc, cnt]))
        P.ts("dve", inc[:], cnt[:], 511.5, step, ALU.is_ge, ALU.mult, reads=[cnt], writes=[inc])
        P.tt("dve", lo[:], lo[:], inc[:], ALU.add, reads=[lo, inc], writes=[lo])
    mo_b = P.sbuf([128, NTO, 16], BF16)
    mo_f = P.sbuf([128, NTO, 16], F32)
    lob = lo[:].unsqueeze(1).to_broadcast([128, NTO, 16])
    P.tt("dve", mo_b[:], affO[:], lob, ALU.is_ge, reads=[affO, lo], writes=[mo_b])
    P.tt("dve", mo_f[:], affO[:], lob, ALU.is_ge, reads=[affO, lo], writes=[mo_f])
    pin = P.psum([128, 512], F32)
    W = NTO * 16
    P.mm(pin[:, 0:W], [(tri_b[:], mo_b[:].rearrange("p n e -> p (n e)"))], reads=[tri_b, mo_b], writes=[pin])
    P.mm(pc[:, 0:W], [(ones_b[:], mo_b[:].rearrange("p n e -> p (n e)"))], reads=[ones_b, mo_b], writes=[pc])
    tot = P.sbuf([128, NTO, 16], F32)
    sa = P.sbuf([128, NTO, 16], F32)
    sb_ = P.sbuf([128, NTO, 16], F32)
    P.copy("dve", tot[:].rearrange("p n e -> p (n e)"), pc[:, 0:W], reads=[pc], writes=[tot])
    P.copy("dve", sa[:], tot[:], reads=[tot], writes=[sa])
    cur, nxt = sa, sb_
    sh = 1
    while sh < NTO:
        P.tt("dve", nxt[:, sh:, :], cur[:, sh:, :], cur[:, :NTO - sh, :], ALU.add, reads=[cur], writes=[nxt])
        P.copy("dve", nxt[:, :sh, :], cur[:, :sh, :], reads=[cur], writes=[nxt])
        cur, nxt = nxt, cur
        sh *= 2
    rank = P.sbuf([128, NTO, 16], F32)
    P.tt("dve", rank[:], cur[:], tot[:], ALU.subtract, reads=[cur, tot], writes=[rank])
    P.tt("dve", rank[:].rearrange("p n e -> p (n e)"), pin[:, 0:W], rank[:].rearrange("p n e -> p (n e)"), ALU.add,
         reads=[pin, rank], writes=[rank])
    sel = P.sbuf([128, NTO, 16], F32)
    P.ts("dve", sel[:], rank[:], 511.5, None, ALU.is_le, reads=[rank], writes=[sel])
    P.tt("dve", sel[:], sel[:], mo_f[:], ALU.mult, reads=[sel, mo_f], writes=[sel])
    slot = P.sbuf([128, NTO, 16], F32)
    P.copy("dve", slot[:], rank[:], reads=[rank], writes=[slot])
    P.tt("dve", slot[:], slot[:], erow.unsqueeze(1).to_broadcast([128, NTO, 16]), ALU.add, reads=[slot, cst], writes=[slot])
    P.ts("dve", slot[:], slot[:], -BIGIDX, None, ALU.add, reads=[slot], writes=[slot])
    P.tt("dve", slot[:], slot[:], sel[:], ALU.mult, reads=[slot, sel], writes=[slot])
    P.ts("dve", slot[:], slot[:], BIGIDX, None, ALU.add, reads=[slot], writes=[slot])
    idx = P.sbuf([128, NTO, 16], U32)
    P.copy("dve", idx[:], slot[:], reads=[slot], writes=[idx])
    gate = P.sbuf([128, NTO, 16], F32)
    P.tt("dve", gate[:], affO[:], sel[:], ALU.mult, reads=[affO, sel], writes=[gate])
    P.dma("sp", idx_out, idx[:], reads=[idx])
    P.dma("sp", gate_out, gate[:], reads=[gate])
    breg = {}

    def bcreg(h):
        if "r" not in breg:
            breg["r"] = h.to_reg(8191)
        return breg["r"]
    for j in range(NTO):
        for e in range(16):
            P.op("pool", (lambda h, j=j, e=e: h.indirect_dma_start(
                out=xs_part[:, :], out_offset=bass.IndirectOffsetOnAxis(ap=idx[:, j, e:e + 1], axis=0),
                in_=u2s[:, j, :], in_offset=None, bounds_check=bcreg(h), oob_is_err=False)),
                _ks([idx, u2s]) + zks, [], dma=True)
    P.emit()
    P.close()


def build_e(nc, cst_ap, wg_ap, wu_ap, wd_ap, xs_ap, ys_out):
    P = PX(nc)
    cst = P.sbuf([128, 512], F32)
    P.dma("sp", cst[:], cst_ap[:, :], writes=[cst])
    ident_b = P.sbuf([128, 128], BF16)
    P.copy("dve", ident_b[:], cst[:, 0:128], reads=[cst], writes=[ident_b])
    units = [dict(wg=P.sbuf([128, 8, 1024], BF16), wu=P.sbuf([128, 8, 1024], BF16), wd=P.sbuf([128, 8, 1024], BF16))
             for _ in range(2)]
    stage = [P.sbuf([128, 2, 1024], F32) for _ in range(4)]
    xa_ring = [P.sbuf([128, 1024], BF16) for _ in range(2)]
    xsT_ring = [P.sbuf([128, 8, 512], BF16) for _ in range(2)]
    hid_ring = [P.sbuf([128, 8, 512], BF16) for _ in range(2)]
    sg_ring = [P.sbuf([128, 512], BF16) for _ in range(2)]
    ys_acc = P.sbuf([128, 4, 1024], F32)
    pg_ring = [P.psum([128, 512], F32) for _ in range(2)]
    pu_ring = [P.psum([128, 512], F32) for _ in range(2)]
    py_ring = [P.psum([128, 512], F32) for _ in range(2)]
    pt = P.psum([128, 1024], BF16)
    sti = [0]

    def chunk_load(U, ci):
        e, h = U // 2, U % 2
        un = units[U % 2]
        which, c2 = ci // 4, ci % 4
        if which == 0:
            src = wg_ap[e].rearrange("(k p) c -> p k c", p=128)[:, 2 * c2:2 * c2 + 2, h * 1024:(h + 1) * 1024]
            dst = un["wg"]
        elif which == 1:
            src = wu_ap[e].rearrange("(k p) c -> p k c", p=128)[:, 2 * c2:2 * c2 + 2, h * 1024:(h + 1) * 1024]
            dst = un["wu"]
        else:
            src = wd_ap[e].rearrange("(k p) c -> p k c", p=128)[:, h * 8 + 2 * c2:h * 8 + 2 * c2 + 2, :]
            dst = un["wd"]
        st = stage[sti[0] % 4]
        sti[0] += 1
        P.dma("sp" if sti[0] % 2 else "pool", st[:], src, writes=[st])
        P.copy("dve" if sti[0] % 2 else "act", dst[:, 2 * c2:2 * c2 + 2, :], st[:], reads=[st], writes=[dst])

    def xs_prep(e):
        xsT = xsT_ring[e % 2]
        for t in range(4):
            xa = xa_ring[t % 2]
            r0 = e * 512 + t * 128
            P.dma("sp", xa[:], xs_ap[r0:r0 + 128, :], writes=[xa])

            def trs(h, xa=xa):
                ins = None
                for k in range(8):
                    ins = h.transpose(out=pt[:, k * 128:(k + 1) * 128], in_=xa[:, k * 128:(k + 1) * 128], identity=ident_b[:])
                return ins
            P.op("pe", trs, _ks([xa, ident_b]), _ks([pt]))
            P.copy("dve" if t % 2 == 0 else "act", xsT[:, :, t * 128:(t + 1) * 128],
                   pt[:].rearrange("p (k t) -> p k t", k=8), reads=[pt], writes=[xsT])

    def compute_steps(U):
        e, h = U // 2, U % 2
        un = units[U % 2]
        xsT = xsT_ring[e % 2]
        hid = hid_ring[U % 2]
        steps = []
        for fc in range(8):
            def st1(fc=fc):
                pg, pu, sg = pg_ring[fc % 2], pu_ring[fc % 2], sg_ring[fc % 2]
                P.mm(pg[:], [(un["wg"][:, k, fc * 128:(fc + 1) * 128], xsT[:, k, :]) for k in range(8)], reads=[un["wg"], xsT], writes=[pg])
                P.mm(pu[:], [(un["wu"][:, k, fc * 128:(fc + 1) * 128], xsT[:, k, :]) for k in range(8)], reads=[un["wu"], xsT], writes=[pu])
                P.act(sg[:], pg[:], AF.Silu, reads=[pg], writes=[sg])
                P.tt("dve", hid[:, fc, :], pu[:], sg[:], ALU.mult, reads=[pu, sg], writes=[hid])
            steps.append(st1)
        for t in range(4):
            for hh in range(2):
                def st2(t=t, hh=hh):
                    py = py_ring[hh]
                    P.mm(py[:], [(hid[:, fc, t * 128:(t + 1) * 128], un["wd"][:, fc, hh * 512:(hh + 1) * 512]) for fc in range(8)],
                         reads=[hid, un["wd"]], writes=[py])
                    if h == 0:
                        P.copy("act", ys_acc[:, t, hh * 512:(hh + 1) * 512], py[:], reads=[py], writes=[ys_acc])
                    else:
                        P.tt("dve", ys_acc[:, t, hh * 512:(hh + 1) * 512], py[:], ys_acc[:, t, hh * 512:(hh + 1) * 512], ALU.add,
                             reads=[py, ys_acc], writes=[ys_acc])
                        if hh == 1:
                            r0 = e * 512 + t * 128
                            P.dma("sp", ys_out[r0:r0 + 128, :], ys_acc[:, t, :], reads=[ys_acc])
                steps.append(st2)
        return steps

    NU = 32
    for ci in range(12):
        chunk_load(0, ci)
    for U in range(NU):
        if U % 2 == 0:
            xs_prep(U // 2)
        steps = compute_steps(U)
        for i, stp in enumerate(steps):
            stp()
            if U + 1 < NU and i < 12:
                chunk_load(U + 1, i)
    P.emit()
    P.close()


def k_inputs(nc):
    d = {}

    def inp(name, shape, dtype=F32):
        d[name] = nc.dram_tensor(name, list(shape), dtype, kind="ExternalInput").ap()
    inp("ysB", [8192, 1024])
    inp("idx", [128, 16, 16], U32)
    inp("gate", [128, 16, 16])
    inp("h1", [2048, 1024])
    inp("cT", [128, 8])
    inp("wmod", [1024, 6144])
    inp("bmod", [6144])
    inp("cst2", [128, 512])
    return d


def combine(P, ys_ap, cst, idx, gate, gtf, h_in_ap, h_out_sink):
    bufs = [P.sbuf([128, 1024], F32) for _ in range(8)]
    for bf in bufs:
        P.memset("pool", bf[:], 0.0, writes=[bf])
    accs = [P.sbuf([128, 1024], F32) for _ in range(2)]
    h_ring = [P.sbuf([128, 1024], F32) for _ in range(2)]
    breg = {}

    def bcreg(h):
        if "r" not in breg:
            breg["r"] = h.to_reg(8191)
        return breg["r"]
    q = 0
    for j in range(NTO):
        acc = accs[j % 2]
        ht = h_ring[j % 2]
        P.dma("sp", ht[:], h_in_ap[j * 128:(j + 1) * 128, :], writes=[ht])
        for e in range(16):
            bf = bufs[q % 8]
            q += 1
            P.op("pool", (lambda h, j=j, e=e, bf=bf: h.indirect_dma_start(
                out=bf[:], out_offset=None, in_=ys_ap[:, :],
                in_offset=bass.IndirectOffsetOnAxis(ap=idx[:, j, e:e + 1], axis=0),
                bounds_check=bcreg(h), oob_is_err=False)), _ks([idx]), _ks([bf]), dma=True)
            if e == 0:
                P.ts("dve", acc[:], bf[:], gate[:, j, e:e + 1], None, ALU.mult, reads=[bf, gate], writes=[acc])
            else:
                P.stt("dve", acc[:], bf[:], gate[:, j, e:e + 1], acc[:], ALU.mult, ALU.add, reads=[bf, gate, acc], writes=[acc])
        P.tt("dve", acc[:], acc[:], gtf[:], ALU.mult, reads=[acc, gtf], writes=[acc])
        P.tt("pool", ht[:], ht[:], acc[:], ALU.add, reads=[ht, acc], writes=[ht])
        h_out_sink(j, ht)


def build_k(nc, I, ys_ap, idx_ap, gate_ap, h_ap, h2_out):
    P = PX(nc)
    cst = P.sbuf([128, 512], F32)
    P.dma("sp", cst[:], I["cst2"][:, :], writes=[cst])
    cactB = load_cact(P, I, cst)
    pbank = [P.psum([128, 512], F32) for _ in range(2)]
    stage = P.sbuf([128, 8, 512], F32)
    gtf = P.sbuf([128, 1024], F32)
    mod_rows(P, nc, cactB, cst[:, 256:384], I["wmod"], I["bmod"], 5120, 1024, pbank, gtf, stage)
    idx = P.sbuf([128, NTO, 16], U32)
    gate = P.sbuf([128, NTO, 16], F32)
    P.dma("sp", idx[:], idx_ap, writes=[idx])
    P.dma("sp", gate[:], gate_ap, writes=[gate])

    def sink(j, ht):
        P.dma("sp", h2_out[j * 128:(j + 1) * 128, :], ht[:], reads=[ht])
    combine(P, ys_ap, cst, idx, gate, gtf, h_ap, sink)
    P.emit()
    P.close()


POOL_W = (2, 4, 8, 16)
POOL_DJ = {2: (-1, 0), 4: (-1, 1), 8: (-2, 2), 16: (-4, 4)}


def pool_ind_list():
    lst = []
    for gi, W in enumerate(POOL_W):
        a, b = POOL_DJ[W]
        for dj in range(a, b + 1):
            lst.append((gi, W, dj))
    return lst


def make_pool_consts():
    lst = pool_ind_list()
    p = np.arange(128)
    ri, ci = p // 64, p % 64
    ind = np.zeros((128, len(lst), 128), np.float32)
    for n, (gi, W, dj) in enumerate(lst):
        dr = 2 * dj + ri[:, None] - ri[None, :]
        dc = ci[:, None] - ci[None, :]
        ind[:, n, :] = ((dr >= -W // 2) & (dr <= W // 2 - 1) & (dc >= -W // 2) & (dc <= W // 2 - 1)).astype(np.float32)
    inv = np.zeros((128, 32, 4), np.float32)
    for io in range(32):
        r = 2 * io + ri
        for gi, W in enumerate(POOL_W):
            cr = np.minimum(r + W - W // 2, 64) - np.maximum(r - W // 2, 0)
            cc = np.minimum(ci + W - W // 2, 64) - np.maximum(ci - W // 2, 0)
            inv[:, io, gi] = 1.0 / (cr * cc).astype(np.float32)
    return ind.astype(ml_dtypes.bfloat16), inv


def build_c1(nc, I, h2_ap, h3_out, u2_out, aff_out):
    P = PX(nc)
    cst = P.sbuf([128, 512], F32)
    P.dma("sp", cst[:], I["cst2"][:, :], writes=[cst])
    ident_f = cst[:, 0:128]
    ones_f = cst[:, 256:384]
    ident_b = P.sbuf([128, 128], BF16)
    P.copy("dve", ident_b[:], ident_f, reads=[cst], writes=[ident_b])
    cactB = load_cact(P, I, cst)
    bankA = P.psum([128, 512], F32)
    bankB = P.psum([128, 512], F32)
    pbank = [bankA, bankB]
    stage = P.sbuf([128, 8, 512], F32)
    B1 = P.sbuf([128, 1024], F32)
    A1 = P.sbuf([128, 1024], F32)
    gtm = P.sbuf([128, 1024], F32)
    tmpv = P.sbuf([128, 1024], F32)
    mod_rows(P, nc, cactB, ones_f, I["wmod"], I["bmod"], 0, 1024, pbank, B1, stage)
    mod_rows(P, nc, cactB, ones_f, I["wmod"], I["bmod"], 1024, 1024, pbank, A1, stage)
    P.dma("sp", tmpv[:], I["gmix"].partition_broadcast(128), writes=[tmpv])
    P.stt("dve", A1[:], A1[:], 1.0, tmpv[:], ALU.add, ALU.mult, reads=[A1, tmpv], writes=[A1])
    mod_rows(P, nc, cactB, ones_f, I["wmod"], I["bmod"], 2048, 1024, pbank, gtm, stage)
    P.dma("sp", tmpv[:], I["pscale"].partition_broadcast(128), reads=[], writes=[tmpv])
    P.tt("dve", gtm[:], gtm[:], tmpv[:], ALU.mult, reads=[gtm, tmpv], writes=[gtm])
    A2, B2 = ffn_mod_setup(P, nc, I, cst, cactB, 1, pbank, stage)
    wr = P.sbuf([128, 8, 16], F32)
    P.dma("sp", wr[:], I["wr"].rearrange("(k p) c -> p k c", p=128), writes=[wr])
    ind = P.sbuf([128, 19, 128], BF16)
    P.dma("sp", ind[:], I["ind"][:, :, :], writes=[ind])
    invc = P.sbuf([128, 32, 4], F32)
    P.dma("sp", invc[:], I["invcnt"][:, :, :], writes=[invc])
    wp_b = P.sbuf([128, 4, 2, 256], BF16)
    wps = P.sbuf([128, 4, 2, 256], F32)
    P.dma("sp", wps[:], I["poolw"].rearrange("g (kk p) e -> p g kk e", p=128), writes=[wps])
    P.copy("pool", wp_b[:], wps[:], reads=[wps], writes=[wp_b])
    u_b = P.sbuf([128, 32, 1024], BF16)
    rs_all = P.sbuf([128, 32], F32)
    hx_ring = [P.sbuf([128, 1024], F32) for _ in range(2)]
    uf_ring = [P.sbuf([128, 1024], F32) for _ in range(2)]
    junk = P.sbuf([128, 1024], BF16)
    ss = P.sbuf([128, 1], F32)
    for i in range(32):
        hx, uf = hx_ring[i % 2], uf_ring[i % 2]
        P.dma("sp", hx[:], h2_ap[i * 128:(i + 1) * 128, :], writes=[hx])
        P.act(junk[:], hx[:], AF.Square, reads=[hx], writes=[junk, ss], accum_out=ss[:])
        P.act(rs_all[:, i:i + 1], ss[:], AF.Sqrt, reads=[ss], writes=[rs_all], scale=1.0 / 1024.0, bias=EPS)
        P.op("dve", (lambda h, i=i: h.reciprocal(out=rs_all[:, i:i + 1], in_=rs_all[:, i:i + 1])), _ks([rs_all]), _ks([rs_all]))
        P.stt("dve", uf[:], hx[:], rs_all[:, i:i + 1], A1[:], ALU.mult, ALU.mult, reads=[hx, rs_all, A1], writes=[uf])
        P.tt("pool", uf[:], uf[:], B1[:], ALU.add, reads=[uf, B1], writes=[uf])
        P.copy("act", u_b[:, i, :], uf[:], reads=[uf], writes=[u_b])
    lst = pool_ind_list()
    aff_sb = P.sbuf([128, NTO, 16], F32)
    rings = make_nr_rings(P)
    pTb = P.psum([128, 1024], BF16)
    py = [P.psum([128, 512], F32) for _ in range(2)]
    p_ring = [P.sbuf([128, 1024], BF16) for _ in range(2)]
    pT_ring = [P.sbuf([128, 1024], BF16) for _ in range(2)]
    h3_ring = [P.sbuf([128, 1024], F32) for _ in range(2)]
    for io in range(32):
        hx, uf, pb_, pTs, h3 = hx_ring[io % 2], uf_ring[io % 2], p_ring[io % 2], pT_ring[io % 2], h3_ring[io % 2]
        P.dma("sp", hx[:], h2_ap[io * 128:(io + 1) * 128, :], writes=[hx])
        P.stt("dve", uf[:], hx[:], rs_all[:, io:io + 1], A1[:], ALU.mult, ALU.mult, reads=[hx, rs_all, A1], writes=[uf])
        P.tt("pool", uf[:], uf[:], B1[:], ALU.add, reads=[uf, B1], writes=[uf])
        for gi in range(4):
            bank = pbank[gi // 2]
            col = (gi % 2) * 256
            pairs = [(ind[:, n, :], u_b[:, io + dj, gi * 256:(gi + 1) * 256]) for n, (g2, W, dj) in enumerate(lst)
                     if g2 == gi and 0 <= io + dj < 32]
            P.mm(bank[:, col:col + 256], pairs, reads=[ind, u_b], writes=[bank])
            P.stt("dve", pb_[:, gi * 256:(gi + 1) * 256], bank[:, col:col + 256], invc[:, io, gi:gi + 1],
                  uf[:, gi * 256:(gi + 1) * 256], ALU.mult, ALU.subtract, reads=[bank, invc, uf], writes=[pb_])

        def trs(h, pb_=pb_):
            ins = None
            for k in range(8):
                ins = h.transpose(out=pTb[:, k * 128:(k + 1) * 128], in_=pb_[:, k * 128:(k + 1) * 128], identity=ident_b[:])
            return ins
        P.op("pe", trs, _ks([pb_, ident_b]), _ks([pTb]))
        P.copy("act", pTs[:], pTb[:], reads=[pTb], writes=[pTs])
        for gi in range(4):
            bank = py[gi // 2]
            col = (gi % 2) * 256
            P.mm(bank[:, col:col + 256], [(pTs[:, (2 * gi + kk) * 128:(2 * gi + kk + 1) * 128], wp_b[:, gi, kk, :]) for kk in range(2)],
                 reads=[pTs, wp_b], writes=[bank])
        for hh in range(2):
            P.tt("dve", h3[:, hh * 512:(hh + 1) * 512], py[hh][:], gtm[:, hh * 512:(hh + 1) * 512], ALU.mult,
                 reads=[py[hh], gtm], writes=[h3])
        P.tt("pool", h3[:], h3[:], hx[:], ALU.add, reads=[h3, hx], writes=[h3])
        P.dma("sp", h3_out[io * 128:(io + 1) * 128, :], h3[:], reads=[h3])
        norm_router(P, I, cst, ident_f, h3, A2, B2, wr, rings, aff_sb, u2_out[io * 128:(io + 1) * 128, :], io)
    P.dma("sp", aff_out.rearrange("(n p) e -> p n e", p=128), aff_sb[:], reads=[aff_sb])
    P.emit()
    P.close()


def kf_inputs(nc):
    d = k_inputs(nc)
    d["fing"] = nc.dram_tensor("fing", [1024], F32, kind="ExternalInput").ap()
    return d


def build_kf(nc, I, ys_ap, idx_ap, gate_ap, h_ap, out_ap):
    P = PX(nc)
    cst = P.sbuf([128, 512], F32)
    P.dma("sp", cst[:], I["cst2"][:, :], writes=[cst])
    cactB = load_cact(P, I, cst)
    pbank = [P.psum([128, 512], F32) for _ in range(2)]
    stage = P.sbuf([128, 8, 512], F32)
    gtf = P.sbuf([128, 1024], F32)
    mod_rows(P, nc, cactB, cst[:, 256:384], I["wmod"], I["bmod"], 5120, 1024, pbank, gtf, stage)
    fg = P.sbuf([128, 1024], F32)
    P.dma("sp", fg[:], I["fing"].partition_broadcast(128), writes=[fg])
    idx = P.sbuf([128, NTO, 16], U32)
    gate = P.sbuf([128, NTO, 16], F32)
    P.dma("sp", idx[:], idx_ap, writes=[idx])
    P.dma("sp", gate[:], gate_ap, writes=[gate])
    junk = P.sbuf([128, 1024], BF16)
    ss = P.sbuf([128, 1], F32)
    rs = P.sbuf([128, 1], F32)
    o_ring = [P.sbuf([128, 1024], F32) for _ in range(2)]

    def sink(j, ht):
        ot = o_ring[j % 2]
        P.act(junk[:], ht[:], AF.Square, reads=[ht], writes=[junk, ss], accum_out=ss[:])
        P.act(rs[:], ss[:], AF.Sqrt, reads=[ss], writes=[rs], scale=1.0 / 1024.0, bias=EPS)
        P.op("dve", (lambda h: h.reciprocal(out=rs[:], in_=rs[:])), _ks([rs]), _ks([rs]))
        P.stt("dve", ot[:], ht[:], rs[:, 0:1], fg[:], ALU.mult, ALU.mult, reads=[ht, rs, fg], writes=[ot])
        P.dma("sp", out_ap[j * 128:(j + 1) * 128, :], ot[:], reads=[ot])
    combine(P, ys_ap, cst, idx, gate, gtf, h_ap, sink)
    P.emit()
    P.close()


GKEYS = ("wqkv", "wz", "wab", "convw", "nalog", "dtb")


def _fused_program():
    nc = bass.Bass("TRN2", target_bir_lowering=False)

    def inp(name, shape, dtype=F32):
        return nc.dram_tensor(name, list(shape), dtype, kind="ExternalInput").ap()

    def scr(name, shape, dtype=F32):
        return nc.dram_tensor(name, list(shape), dtype).ap()
    x = inp("x", [4352, 1024])
    cT = inp("cT", [128, 8, 2])
    cT1 = inp("cT1", [128, 8])
    wmod = [inp("wmod0", [1024, 6144]), inp("wmod1", [1024, 6144])]
    bmod = [inp("bmod0", [6144]), inp("bmod1", [6144])]
    bmodg = inp("bmodg", [128, 16])
    gmixg = inp("gmixg", [128, 8])
    gmix1 = inp("gmix1", [1024])
    gffn = [inp("gffn0", [1024]), inp("gffn1", [1024])]
    ogain = inp("ogain", [128, 1])
    cst = inp("cst", [128, 768])
    cst2 = inp("cst2", [128, 512])
    shapes = {"wqkv": [1024, 1536], "wz": [1024, 512], "wab": [1024, 16], "convw": [128, 12, 5], "nalog": [128, 16], "dtb": [128, 16]}
    grp = [{k: inp(f"{k}{g}", shapes[k]) for k in GKEYS} for g in range(2)]
    wout = inp("wout", [1024, 1024])
    wr = [inp("wr0", [1024, 16]), inp("wr1", [1024, 16])]
    ind = inp("ind", [128, 19, 128], BF16)
    invcnt = inp("invcnt", [128, 32, 4])
    poolw = inp("poolw", [4, 256, 256])
    pscale = inp("pscale", [1024])
    fing = inp("fing", [1024])
    wg = [inp("wg0", [16, 1024, 2048]), inp("wg1", [16, 1024, 2048])]
    wu = [inp("wu0", [16, 1024, 2048]), inp("wu1", [16, 1024, 2048])]
    wd = [inp("wd0", [16, 2048, 1024]), inp("wd1", [16, 2048, 1024])]
    out = nc.dram_tensor("out", [4096, 1024], F32, kind="ExternalOutput").ap()

    qkv_d = scr("qkv_d", [4, 3, 128, 4352], BF16)
    zT_d = scr("zT_d", [4, 128, 4096], BF16)
    gb_d = scr("gb_d", [34, 128, 16])
    ogT_d = scr("ogT_d", [1024, 4096], BF16)
    h1_d = scr("h1_d", [4096, 1024])
    u2_d = scr("u2_d", [4096, 1024], BF16)
    aff_d = scr("aff_d", [4096, 16])
    xs_d = scr("xs_d", [8192, 1024], BF16)
    idx_d = scr("idx_d", [128, 32, 16], U32)
    gate_d = scr("gate_d", [128, 32, 16])
    ys_d = scr("ys_d", [8192, 1024])
    h2_d = scr("h2_d", [4096, 1024])
    h3_d = scr("h3_d", [4096, 1024])

    import os
    PH = int(os.environ.get("FUSED_PH", "99"))
    for g in range(2):
        if g > PH:
            break
        Ig = {"x": x, "cT": cT, "wmod": wmod[0], "bmod": bmodg, "gmix": gmixg, "ogain": ogain, "cst": cst}
        Ig.update(grp[g])
        build_g1(nc, Ig, qkv_d, zT_d, gb_d)
        build_g2(nc, Ig, qkv_d, zT_d, gb_d, ogT_d[g * 512:(g + 1) * 512, :])
    Im = [{"cT": cT1, "wmod": wmod[l], "bmod": bmod[l], "gffn": gffn[l], "wout": wout, "wr": wr[l], "cst2": cst2,
           "gmix": gmix1, "pscale": pscale, "ind": ind, "invcnt": invcnt, "poolw": poolw, "fing": fing} for l in range(2)]
    if PH >= 2:
        build_c0(nc, Im[0], ogT_d, x[256:4352, :], h1_d, u2_d, aff_d)
    if PH >= 3:
        build_r(nc, Im[0], aff_d, u2_d, xs_d, idx_d, gate_d)
    if PH >= 4:
        build_e(nc, cst2, wg[0], wu[0], wd[0], xs_d, ys_d)
    if PH >= 5:
        build_k(nc, Im[0], ys_d, idx_d, gate_d, h1_d, h2_d)
    if PH >= 6:
        build_c1(nc, Im[1], h2_d, h3_d, u2_d, aff_d)
    if PH >= 7:
        build_r(nc, Im[1], aff_d, u2_d, xs_d, idx_d, gate_d)
    if PH >= 8:
        build_e(nc, cst2, wg[1], wu[1], wd[1], xs_d, ys_d)
    if PH >= 9:
        build_kf(nc, Im[1], ys_d, idx_d, gate_d, h3_d, out)
    return nc


def _host_inputs(inp, b):
    d = {}
    g0 = g1_host_inputs(inp, 2 * b)
    g1 = g1_host_inputs(inp, 2 * b + 1)
    d["x"] = g0["x"]
    d["cT"] = g0["cT"]
    d["cT1"] = np.ascontiguousarray(inp["c"][b].reshape(8, 128).T)
    d["bmodg"] = g0["bmod"]
    d["gmixg"] = g0["gmix"]
    d["ogain"] = g0["ogain"]
    d["cst"] = g0["cst"]
    for k in GKEYS:
        d[k + "0"] = g0[k]
        d[k + "1"] = g1[k]
    for l in range(2):
        d[f"wmod{l}"] = np.ascontiguousarray(inp["w_mod"][l])
        d[f"bmod{l}"] = np.ascontiguousarray(inp["b_mod"][l])
        d[f"gffn{l}"] = np.ascontiguousarray(inp["norm_ffn_g"][l])
        d[f"wr{l}"] = np.ascontiguousarray(inp["moe_w_router"][l])
        d[f"wg{l}"] = np.ascontiguousarray(inp["moe_w_gate"][l])
        d[f"wu{l}"] = np.ascontiguousarray(inp["moe_w_up"][l])
        d[f"wd{l}"] = np.ascontiguousarray(inp["moe_w_down"][l])
    d["gmix1"] = np.ascontiguousarray(inp["norm_mix_g"][1])
    d["cst2"] = make_cst2()
    d["wout"] = np.ascontiguousarray(inp["gdn_w_out"][0])
    ind, inv = make_pool_consts()
    d["ind"], d["invcnt"] = ind, inv
    d["poolw"] = np.ascontiguousarray(inp["pool_w"][0])
    d["pscale"] = np.ascontiguousarray(inp["pool_scale"][0])
    d["fing"] = np.ascontiguousarray(inp["final_g"])
    return d


def kernel(**inputs):
    inp = {k: np.asarray(v) for k, v in inputs.items()}
    nc = _fused_program()
    maps = []
    for b in range(4):
        m = _host_inputs(inp, b)
        maps.append(m)
        maps.append(m)
    res = run_bass_kernel_spmd(nc, maps, core_ids=list(range(8))).results
    out = np.stack([np.concatenate([np.asarray(res[2 * b]["out"])[0:2048], np.asarray(res[2 * b + 1]["out"])[2048:4096]], axis=0)
                    for b in range(4)])
    return out.astype(np.float32)
```

```python
import ml_dtypes
from concourse.bass_utils import run_bass_kernel_spmd
from contextlib import ExitStack
import numpy as np
import concourse.bass as bass
import concourse.mybir as mybir

F32 = mybir.dt.float32
BF16 = mybir.dt.bfloat16
I32 = mybir.dt.int32
U32 = mybir.dt.uint32
AF = mybir.ActivationFunctionType
ALU = mybir.AluOpType
AX = mybir.AxisListType


class Tk:
    __slots__ = ("w", "r", "rd")

    def __init__(self):
        self.w = None
        self.r = {}
        self.rd = []


class Op:
    __slots__ = ("fn", "deps", "dma", "signal", "dmaid")

    def __init__(self, fn, deps, dma):
        self.fn = fn
        self.deps = deps
        self.dma = dma
        self.signal = False
        self.dmaid = None


class Prog:
    ENG = ("pe", "act", "dve", "pool", "sp")
    NDS = 8
    _uid = 0
    _g = {}
    _tn = 0

    def __init__(self, nc):
        self.nc = nc
        self.es = ExitStack()
        self.ops = {e: [] for e in self.ENG}
        self.ndma = {e: 0 for e in self.ENG}
        self.dma_of = {}
        self._n = 0

    def sb(self, shape, dtype, name=None):
        Prog._tn += 1
        name = name or f"sb{Prog._tn}"
        return self.es.enter_context(self.nc.sbuf_tensor(name, list(shape), dtype))

    def ps(self, shape, dtype=F32, name=None):
        Prog._tn += 1
        name = name or f"ps{Prog._tn}"
        return self.es.enter_context(self.nc.psum_tensor(name, list(shape), dtype))

    def tk(self):
        return Tk()

    def op(self, eng, fn, reads=(), writes=(), dma=False):
        idx = len(self.ops[eng])
        me = ("dma", eng, self.ndma[eng]) if dma else (eng, idx)
        deps = set()
        for t in reads:
            if t.w is not None:
                deps.add(t.w)
        for t in writes:
            if t.w is not None:
                deps.add(t.w)
            for e2, j in t.r.items():
                deps.add((e2, j))
            for d in t.rd:
                deps.add(d)
        for t in reads:
            if dma:
                t.rd.append(me)
            else:
                if t.r.get(eng, -1) < idx:
                    t.r[eng] = idx
        for t in writes:
            t.w = me
            t.r = {}
            t.rd = []
        deps.discard(me)
        if eng == "pe" and not dma:
            deps = {d for d in deps if not (d[0] == "pe")}
        o = Op(fn, deps, dma)
        if dma:
            o.dmaid = me
            self.ndma[eng] += 1
        self.ops[eng].append(o)
        return me

    def emit(self):
        nc = self.nc
        for e in self.ENG:
            for o in self.ops[e]:
                for d in o.deps:
                    if d[0] != "dma":
                        self.ops[d[0]][d[1]].signal = True
        for e in self.ENG:
            for o in reversed(self.ops[e]):
                if not o.dma:
                    o.signal = True
                    break
        sigcount = {}
        for e in self.ENG:
            c = 0
            lst = []
            for o in self.ops[e]:
                if o.signal and not o.dma:
                    c += 1
                lst.append(c)
            sigcount[e] = lst
        G = Prog._g
        if G.get("nc") is not nc:
            G.clear()
            G["nc"] = nc
            G["s"] = {e: nc.alloc_semaphore(name=f"s_{e}") for e in self.ENG}
            G["sb"] = {e: 0 for e in self.ENG}
            G["bar"] = nc.alloc_semaphore(name="bar")
            G["barb"] = 0
            G["d"] = {}
            G["db"] = {}
        G["np"] = G.get("np", 0) + 1
        sems = {e: nc.alloc_semaphore(name=f"s{G['np']}_{e}") for e in self.ENG}
        sb = {e: 0 for e in self.ENG}
        bar = G["bar"]
        barb = G["barb"]
        NDS = self.NDS
        for e in self.ENG:
            if self.ndma[e] > 0 and e not in G["d"]:
                G["d"][e] = [nc.alloc_semaphore(name=f"d_{e}{i}") for i in range(NDS)]
                for i in range(NDS):
                    G["db"][(e, i)] = 0
        dsems = G["d"]
        db = dict(G["db"])

        def run(e, h):
            waited = {}
            waited_d = {}
            for i, o in enumerate(self.ops[e]):
                for d in sorted(o.deps, key=str):
                    if d[0] == "dma":
                        _, e2, k = d
                        s = dsems[e2][k % NDS]
                        v = db[(e2, k % NDS)] + 16 * (k // NDS + 1)
                        key = (e2, k % NDS)
                        if waited_d.get(key, 0) < v:
                            h.wait_ge(s, v)
                            waited_d[key] = v
                    else:
                        e2, j = d
                        c = sb[e2] + sigcount[e2][j]
                        if waited.get(e2, 0) < c:
                            h.wait_ge(sems[e2], c)
                            waited[e2] = c
                if o.dma:
                    k = o.dmaid[2]
                    s = dsems[e][k % NDS]
                    if k >= NDS:
                        v = db[(e, k % NDS)] + 16 * (k // NDS)
                        key = (e, k % NDS)
                        if waited_d.get(key, 0) < v:
                            h.wait_ge(s, v)
                            waited_d[key] = v
                    ins = o.fn(h)
                    ins.then_inc(s, 16)
                else:
                    ins = o.fn(h)
                    if o.signal:
                        ins.then_inc(sems[e], 1)
            if self.ndma[e] > 0:
                n = self.ndma[e]
                for q in range(min(NDS, n)):
                    cnt = (n - 1 - q) // NDS + 1
                    h.wait_ge(dsems[e][q], db[(e, q)] + 16 * cnt)
            if sigcount[e] and sigcount[e][-1] > 0:
                h.wait_ge(sems[e], sb[e] + sigcount[e][-1])
            h.sem_inc(bar, 1)
            h.wait_ge(bar, barb + 5)

        with nc.Block() as block:
            block.sync(lambda h: run("sp", h))
            block.tensor(lambda h: run("pe", h))
            block.scalar(lambda h: run("act", h))
            block.vector(lambda h: run("dve", h))
            block.gpsimd(lambda h: run("pool", h))
        for e in self.ENG:
            n = self.ndma[e]
            for q in range(min(NDS, n)):
                G["db"][(e, q)] += 16 * ((n - 1 - q) // NDS + 1)
        G["barb"] += 5

    def close(self):
        self.es.close()


class Buf:
    __slots__ = ("t", "k", "ps")

    def __init__(self, t, ps=False):
        self.t = t
        self.k = Tk()
        self.ps = ps

    def __getitem__(self, idx):
        return self.t[idx]


def _ks(bufs):
    return [b.k if isinstance(b, Buf) else b for b in bufs]


def _rw(reads, writes):
    r, w = [], []
    for b in reads:
        if isinstance(b, Buf) and b.ps:
            w.append(b.k)
        else:
            r.append(b.k if isinstance(b, Buf) else b)
    for b in writes:
        w.append(b.k if isinstance(b, Buf) else b)
    return r, w


class PX(Prog):
    def sbuf(self, shape, dtype, name=None):
        return Buf(self.sb(shape, dtype, name))

    def psum(self, shape, dtype=F32, name=None):
        return Buf(self.ps(shape, dtype, name), ps=True)

    def dma(self, q, out, in_, reads=(), writes=()):
        return self.op(q, lambda h: h.dma_start(out=out, in_=in_), *_rw(reads, writes), dma=True)

    def mm(self, out, pairs, reads=(), writes=()):
        n = len(pairs)

        def f(h):
            ins = None
            for i, (l, r) in enumerate(pairs):
                ins = h.matmul(out, lhsT=l, rhs=r, start=(i == 0), stop=(i == n - 1))
            return ins
        return self.op("pe", f, *_rw(reads, writes))

    def tr(self, out, in_, ident, reads=(), writes=()):
        return self.op("pe", lambda h: h.transpose(out=out, in_=in_, identity=ident), *_rw(reads, writes))

    def act(self, out, in_, func, reads=(), writes=(), **kw):
        return self.op("act", lambda h: h.activation(out=out, in_=in_, func=func, **kw), *_rw(reads, writes))

    def ts(self, eng, out, in0, s1, s2, op0, op1=None, reads=(), writes=(), **kw):
        if op1 is None:
            return self.op(eng, lambda h: h.tensor_scalar(out=out, in0=in0, scalar1=s1, scalar2=None, op0=op0, **kw),
                           *_rw(reads, writes))
        return self.op(eng, lambda h: h.tensor_scalar(out=out, in0=in0, scalar1=s1, scalar2=s2, op0=op0, op1=op1, **kw),
                       *_rw(reads, writes))

    def tt(self, eng, out, in0, in1, op, reads=(), writes=()):
        return self.op(eng, lambda h: h.tensor_tensor(out=out, in0=in0, in1=in1, op=op), *_rw(reads, writes))

    def stt(self, eng, out, in0, scalar, in1, op0, op1, reads=(), writes=()):
        return self.op(eng, lambda h: h.scalar_tensor_tensor(out=out, in0=in0, scalar=scalar, in1=in1, op0=op0, op1=op1),
                       *_rw(reads, writes))

    def copy(self, eng, out, in_, reads=(), writes=()):
        if eng == "act":
            return self.op("act", lambda h: h.copy(out=out, in_=in_), *_rw(reads, writes))
        return self.op(eng, lambda h: h.tensor_copy(out=out, in_=in_), *_rw(reads, writes))

    def memset(self, eng, ap, val, writes=()):
        return self.op(eng, lambda h: h.memset(ap, val), (), _ks(writes))


NT = 34
TP = 4358
EPS = 1e-6
NEGBIG = -30000.0


def pad_pos(tok):
    return tok + 2 if tok < 256 else tok + 4


def g1_inputs(nc):
    d = {}

    def inp(name, shape, dtype=F32):
        d[name] = nc.dram_tensor(name, list(shape), dtype, kind="ExternalInput").ap()
    inp("x", [4352, 1024])
    inp("cT", [128, 8, 2])
    inp("wmod", [1024, 2048])
    inp("bmod", [128, 16])
    inp("gmix", [128, 8])
    inp("wqkv", [1024, 1536])
    inp("wz", [1024, 512])
    inp("wab", [1024, 16])
    inp("convw", [128, 12, 5])
    inp("nalog", [128, 16])
    inp("dtb", [128, 16])
    inp("ogain", [128, 1])
    inp("cst", [128, 6 * 128])
    return d


def build_g1(nc, I, qkv_d, zT_d, gb_d):
    P = PX(nc)
    cst = P.sbuf([128, 6 * 128], F32)
    P.dma("sp", cst[:], I["cst"][:, :], writes=[cst])
    ident_f = cst[:, 0:128]
    ones_f = cst[:, 5 * 128:6 * 128]
    ident_b = P.sbuf([128, 128], BF16)
    P.copy("dve", ident_b[:], ident_f, reads=[cst], writes=[ident_b])

    cT = P.sbuf([128, 8, 2], F32)
    P.dma("sp", cT[:], I["cT"][:, :, :], writes=[cT])
    cact = P.sbuf([128, 8, 2], F32)
    P.act(cact[:], cT[:], AF.Silu, reads=[cT], writes=[cact])
    bmod = P.sbuf([128, 16], F32)
    P.dma("sp", bmod[:], I["bmod"][:, :], writes=[bmod])
    gmix = P.sbuf([128, 8], F32)
    P.dma("sp", gmix[:], I["gmix"][:, :], writes=[gmix])
    wm_ring = [P.sbuf([128, 8, 128], F32) for _ in range(3)]
    pp_ring = [P.psum([128, 512], F32) for _ in range(2)]
    pss_ring = [P.psum([128, 512], F32) for _ in range(2)]
    ps_mod = Buf(pp_ring[0][:, 0:32].rearrange("p (j s) -> p j s", s=2), ps=True)
    ps_mod.k = pp_ring[0].k
    wmod_v = I["wmod"].rearrange("(k p) c -> p k c", p=128)
    for j in range(16):
        wm = wm_ring[j % 3]
        P.dma("sp", wm[:], wmod_v[:, :, j * 128:(j + 1) * 128], writes=[wm])
        P.mm(ps_mod.t[:, j, :], [(wm[:, k, :], cact[:, k, :]) for k in range(8)], reads=[wm, cact], writes=[ps_mod])
    modc = P.sbuf([128, 16, 2], F32)
    for s in range(2):
        P.tt("dve", modc[:, :, s], ps_mod.t[:, :, s], bmod[:], ALU.add, reads=[ps_mod, bmod], writes=[modc])
    Acol = P.sbuf([128, 8, 2], F32)
    for s in range(2):
        P.stt("dve", Acol[:, :, s], modc[:, 8:16, s], 1.0, gmix[:], ALU.add, ALU.mult, reads=[modc, gmix], writes=[Acol])

    wqkv_b = P.sbuf([128, 8, 1536], BF16)
    wz_b = P.sbuf([128, 8, 512], BF16)
    wab_b = P.sbuf([128, 8, 16], BF16)
    stage = [P.sbuf([128, 2064], F32) for _ in range(2)]
    wq_v = I["wqkv"].rearrange("(k p) c -> p k c", p=128)
    wz_v = I["wz"].rearrange("(k p) c -> p k c", p=128)
    wab_v = I["wab"].rearrange("(k p) c -> p k c", p=128)
    for k in range(8):
        st = stage[k % 2]
        P.dma("sp", st[:, 0:1536], wq_v[:, k, :], writes=[st])
        P.dma("sp", st[:, 1536:2048], wz_v[:, k, :], writes=[st])
        P.dma("sp", st[:, 2048:2064], wab_v[:, k, :], writes=[st])
        P.copy("pool", wqkv_b[:, k, :], st[:, 0:1536], reads=[st], writes=[wqkv_b])
        P.copy("pool", wz_b[:, k, :], st[:, 1536:2048], reads=[st], writes=[wz_b])
        P.copy("pool", wab_b[:, k, :], st[:, 2048:2064], reads=[st], writes=[wab_b])
    convw = P.sbuf([128, 12, 5], F32)
    P.dma("sp", convw[:], I["convw"][:, :, :], writes=[convw])

    uT = P.sbuf([128, 8, TP], BF16)
    for (a, b) in ((0, 2), (258, 260), (4356, 4358)):
        P.memset("pool", uT[:, :, a:b], 0.0, writes=[uT])
    xt_ring = [P.sbuf([128, 1024], F32) for _ in range(3)]
    xn_ring = [P.sbuf([128, 1024], BF16) for _ in range(2)]
    junk = P.sbuf([128, 1024], BF16)
    ss_ring = [P.sbuf([128, 1], F32) for _ in range(3)]
    rs_ring = [P.sbuf([128, 1], F32) for _ in range(3)]
    ptr_ring = [P.psum([128, 8, 128], BF16) for _ in range(2)]
    for i in range(NT):
        xt = xt_ring[i % 3]
        xn = xn_ring[i % 2]
        ss = ss_ring[i % 3]
        rs = rs_ring[i % 3]
        ptr = ptr_ring[i % 2]
        P.dma("sp", xt[:], I["x"][i * 128:(i + 1) * 128, :], writes=[xt])
        P.act(junk[:], xt[:], AF.Square, reads=[xt], writes=[junk, ss], accum_out=ss[:])
        P.act(rs[:], ss[:], AF.Sqrt, reads=[ss], writes=[rs], scale=1.0 / 1024.0, bias=EPS)
        P.op("dve", (lambda h, rs=rs: h.reciprocal(out=rs[:], in_=rs[:])), _ks([rs]), _ks([rs]))
        P.act(xn[:], xt[:], AF.Copy, reads=[xt, rs], writes=[xn], scale=rs[:, 0:1])

        def trs(h, xn=xn, ptr=ptr):
            ins = None
            for k in range(8):
                ins = h.transpose(out=ptr[:, k, :], in_=xn[:, k * 128:(k + 1) * 128], identity=ident_b[:])
            return ins
        P.op("pe", trs, _ks([xn, ident_b]), _ks([ptr]))
        s = 1 if i < 2 else 0
        p0 = pad_pos(i * 128)
        for k in range(8):
            eng = "dve" if k % 2 == 0 else "act"
            if eng == "dve":
                P.ts("dve", uT[:, k, p0:p0 + 128], ptr[:, k, :], Acol[:, k, s:s + 1], modc[:, k, s:s + 1],
                     ALU.mult, ALU.add, reads=[ptr, Acol, modc], writes=[uT])
            else:
                P.act(uT[:, k, p0:p0 + 128], ptr[:, k, :], AF.Identity, reads=[ptr, Acol, modc], writes=[uT],
                      scale=Acol[:, k, s:s + 1], bias=modc[:, k, s:s + 1])

    nalog = P.sbuf([128, 16], F32)
    dtb = P.sbuf([128, 16], F32)
    P.dma("sp", nalog[:], I["nalog"][:, :], writes=[nalog])
    P.dma("sp", dtb[:], I["dtb"][:, :], writes=[dtb])
    nA = P.sbuf([128, 16], F32)
    P.act(nA[:], nalog[:], AF.Exp, reads=[nalog], writes=[nA])
    P.ts("dve", nA[:], nA[:], -1.0, None, ALU.mult, reads=[nA], writes=[nA])
    gb = P.sbuf([128, NT, 16], F32)
    ps_ab = []
    for r in pss_ring:
        bb = Buf(r[:, 0:272].rearrange("p (n c) -> p n c", c=16), ps=True)
        bb.k = r.k
        ps_ab.append(bb)
    for i in range(NT):
        p0 = pad_pos(i * 128)
        pab = ps_ab[i // 17]
        P.mm(pab.t[:, i % 17, :], [(uT[:, k, p0:p0 + 128], wab_b[:, k, :]) for k in range(8)],
             reads=[uT, wab_b], writes=[pab])
    tmp_ab = P.sbuf([128, NT, 16], F32)
    for hf in range(2):
        sl = slice(hf * 17, hf * 17 + 17)
        P.tt("dve", tmp_ab[:, sl, :], ps_ab[hf].t, dtb[:].unsqueeze(1).to_broadcast([128, 17, 16]), ALU.add,
             reads=[ps_ab[hf], dtb], writes=[tmp_ab])
    tv = tmp_ab[:].rearrange("p n (d w h) -> p n d w h", d=2, w=2)
    gv = gb[:].rearrange("p n (d w h) -> p n d w h", d=2, w=2)
    nAv = nA[:].rearrange("p (d w h) -> p d w h", d=2, w=2)
    e_ab = P.sbuf([128, NT, 16], F32)
    ev = e_ab[:].rearrange("p n (d w h) -> p n d w h", d=2, w=2)
    for d in range(2):
        P.act(ev[:, :, d, 0, :], tv[:, :, d, 0, :], AF.Exp, reads=[tmp_ab], writes=[e_ab])
    for d in range(2):
        P.act(ev[:, :, d, 0, :], ev[:, :, d, 0, :], AF.Ln, reads=[e_ab], writes=[e_ab], bias=1.0)
    for d in range(2):
        P.tt("dve", gv[:, :, d, 0, :], ev[:, :, d, 0, :], nAv[:, d, 0, :].unsqueeze(1).to_broadcast([128, NT, 4]),
             ALU.mult, reads=[e_ab, nA], writes=[gb])
    for d in range(2):
        P.act(gv[:, :, d, 1, :], tv[:, :, d, 1, :], AF.Sigmoid, reads=[tmp_ab], writes=[gb])
    P.dma("sp", gb_d.rearrange("n p c -> p n c"), gb[:], reads=[gb])

    NW = 9
    pb_ring = [P.sbuf([128, 512], F32) for _ in range(2)]
    aA_ring = [P.sbuf([128, 512], F32) for _ in range(2)]
    aB_ring = [P.sbuf([128, 512], F32) for _ in range(2)]
    s_ring = [P.sbuf([128, 512], F32) for _ in range(2)]
    sq_ring = [P.sbuf([128, 512], F32) for _ in range(2)]
    ri_ring = [P.sbuf([128, 512], F32) for _ in range(2)]
    ob_ring = [P.sbuf([128, 512], BF16) for _ in range(3)]
    it = 0
    items = [(j, w) for j in range(12) for w in range(NW)]

    def stageA(n):
        j, w = items[n]
        which = j % 3
        c0 = 508 * w
        L = min(512, TP - c0)
        Lo = L - 4
        pp = pp_ring[n % 2]
        pb, aA, aB, sb_, sq = (r[n % 2] for r in (pb_ring, aA_ring, aB_ring, s_ring, sq_ring))
        ob = ob_ring[n % 3]
        P.mm(pp[:, 0:L], [(wqkv_b[:, k, j * 128:(j + 1) * 128], uT[:, k, c0:c0 + L]) for k in range(8)],
             reads=[wqkv_b, uT], writes=[pp])
        P.act(aA[:, 0:Lo], pp[:, 0:Lo], AF.Copy, reads=[pp, convw], writes=[aA], scale=convw[:, j, 0:1])
        P.act(pb[:, 0:L], pp[:, 0:L], AF.Copy, reads=[pp], writes=[pb])
        P.stt("dve", aA[:, 0:Lo], pb[:, 1:1 + Lo], convw[:, j, 1:2], aA[:, 0:Lo], ALU.mult, ALU.add,
              reads=[pb, convw, aA], writes=[aA])
        P.stt("dve", aA[:, 0:Lo], pb[:, 2:2 + Lo], convw[:, j, 2:3], aA[:, 0:Lo], ALU.mult, ALU.add,
              reads=[pb, convw, aA], writes=[aA])
        P.act(aB[:, 0:Lo], pb[:, 3:3 + Lo], AF.Copy, reads=[pb, convw], writes=[aB], scale=convw[:, j, 3:4])
        P.stt("dve", aB[:, 0:Lo], pb[:, 4:4 + Lo], convw[:, j, 4:5], aB[:, 0:Lo], ALU.mult, ALU.add,
              reads=[pb, convw, aB], writes=[aB])
        P.tt("pool", aA[:, 0:Lo], aA[:, 0:Lo], aB[:, 0:Lo], ALU.add, reads=[aA, aB], writes=[aA])
        if which == 2:
            P.act(ob[:, 0:Lo], aA[:, 0:Lo], AF.Silu, reads=[aA], writes=[ob])
        else:
            P.act(sb_[:, 0:Lo], aA[:, 0:Lo], AF.Silu, reads=[aA], writes=[sb_])
            P.tt("pool", sq[:, 0:Lo], sb_[:, 0:Lo], sb_[:, 0:Lo], ALU.mult, reads=[sb_], writes=[sq])

    def stageB(n):
        j, w = items[n]
        hl, which = j // 3, j % 3
        c0 = 508 * w
        L = min(512, TP - c0)
        Lo = L - 4
        pss = pss_ring[n % 2]
        sb_, sq, ri = (r[n % 2] for r in (s_ring, sq_ring, ri_ring))
        ob = ob_ring[n % 3]
        if which != 2:
            P.mm(pss[:, 0:Lo], [(ones_f, sq[:, 0:Lo])], reads=[cst, sq], writes=[pss])
            P.act(ri[:, 0:Lo], pss[:, 0:Lo], AF.Sqrt, reads=[pss], writes=[ri], bias=EPS)
            P.op("dve", (lambda h, ri=ri, Lo=Lo: h.reciprocal(out=ri[:, 0:Lo], in_=ri[:, 0:Lo])), _ks([ri]), _ks([ri]))
            sc = (128.0 ** -0.5) if which == 0 else 1.0
            P.stt("dve", ob[:, 0:Lo], sb_[:, 0:Lo], sc, ri[:, 0:Lo], ALU.mult, ALU.mult, reads=[sb_, ri], writes=[ob])
        dst = qkv_d[hl, which]
        if w == 0:
            P.dma("sp", dst[:, 0:256], ob[:, 0:256], reads=[ob])
            P.dma("sp", dst[:, 256:256 + (Lo - 258)], ob[:, 258:Lo], reads=[ob])
        else:
            t0 = c0 + 2 - 4
            P.dma("sp", dst[:, t0:t0 + Lo], ob[:, 0:Lo], reads=[ob])

    for n in range(len(items) + 1):
        if n < len(items):
            stageA(n)
        if n >= 1:
            stageB(n - 1)
    it = len(items)

    for hl in range(4):
        for gq in range(8):
            pp = pp_ring[it % 2]
            ob = ob_ring[it % 3]
            it += 1
            c0 = 260 + 512 * gq
            P.mm(pp[:], [(wz_b[:, k, hl * 128:(hl + 1) * 128], uT[:, k, c0:c0 + 512]) for k in range(8)],
                 reads=[wz_b, uT], writes=[pp])
            P.act(ob[:], pp[:], AF.Silu, reads=[pp], writes=[ob])
            P.dma("sp", zT_d[hl][:, gq * 512:(gq + 1) * 512], ob[:], reads=[ob])
    P.emit()
    P.close()


def g1_host_inputs(inp, core):
    b, g = core // 2, core % 2
    heads = [4 * g + i for i in range(4)]
    d = {}
    d["x"] = np.ascontiguousarray(np.concatenate([inp["ctx"][b], inp["x"][b]], axis=0))
    cT = np.zeros((128, 8, 2), np.float32)
    cT[:, :, 0] = inp["c"][b].reshape(8, 128).T
    cT[:, :, 1] = inp["c_ctx"].reshape(8, 128).T
    d["cT"] = cT
    d["wmod"] = np.ascontiguousarray(inp["w_mod"][0][:, 0:2048])
    d["bmod"] = np.ascontiguousarray(inp["b_mod"][0][0:2048].reshape(16, 128).T)
    d["gmix"] = np.ascontiguousarray(inp["norm_mix_g"][0].reshape(8, 128).T)
    w_in = inp["gdn_w_in"][0]
    cols = []
    for hl, h in enumerate(heads):
        for which in range(3):
            cols.append(np.arange(which * 1024 + h * 128, which * 1024 + (h + 1) * 128))
    cols = np.concatenate(cols)
    d["wqkv"] = np.ascontiguousarray(w_in[:, cols])
    zc = np.concatenate([np.arange(3072 + h * 128, 3072 + (h + 1) * 128) for h in heads])
    d["wz"] = np.ascontiguousarray(w_in[:, zc])
    abc = np.array([4096 + dd * 16 + w * 8 + h for dd in range(2) for w in range(2) for h in heads])
    d["wab"] = np.ascontiguousarray(w_in[:, abc])
    cw = inp["gdn_conv_w"][0][:, cols]
    d["convw"] = np.ascontiguousarray(cw.reshape(5, 12, 128).transpose(2, 1, 0))
    nalog = np.zeros((128, 16), np.float32)
    dtb = np.zeros((128, 16), np.float32)
    for dd in range(2):
        for hl, h in enumerate(heads):
            nalog[:, dd * 8 + hl] = inp["gdn_a_log"][0][dd, h]
            dtb[:, dd * 8 + hl] = inp["gdn_dt_bias"][0][dd, h]
    d["nalog"] = nalog
    d["dtb"] = dtb
    d["ogain"] = np.ascontiguousarray(inp["gdn_o_gain"][0].reshape(128, 1))
    d["cst"] = make_cst()
    return d


def make_cst():
    i = np.arange(128)
    ident = np.eye(128, dtype=np.float32)
    tri0 = (i[:, None] <= i[None, :]).astype(np.float32)
    tri1 = (i[:, None] >= i[None, :]).astype(np.float32)
    negS0 = np.where(i[None, :] > i[:, None], 0.0, NEGBIG).astype(np.float32)
    negS1 = np.where(i[None, :] < i[:, None], 0.0, NEGBIG).astype(np.float32)
    ones = np.ones((128, 128), np.float32)
    return np.ascontiguousarray(np.concatenate([ident, tri0, tri1, negS0, negS1, ones], axis=1))


def tile_of(d, s):
    if d == 0:
        return s
    return (1 - s) if s < 2 else 35 - s


def build_g2(nc, I, qkv_d, zT_d, gb_d, ogT_out, nsteps=34, ngate=4):
    P = PX(nc)
    cst = P.sbuf([128, 6 * 128], F32)
    P.dma("sp", cst[:], I["cst"][:, :], writes=[cst])
    ident_f = cst[:, 0:128]
    tri = [cst[:, 128:256], cst[:, 256:384]]
    negS = [cst[:, 384:512], cst[:, 512:640]]
    ones_f = cst[:, 640:768]
    ident_b = P.sbuf([128, 128], BF16)
    P.copy("dve", ident_b[:], ident_f, reads=[cst], writes=[ident_b])
    ogain = P.sbuf([128, 1], F32)
    P.dma("sp", ogain[:], I["ogain"][:, :], writes=[ogain])
    gb = P.sbuf([128, NT, 16], F32)
    P.dma("sp", gb[:], gb_d.rearrange("n p c -> p n c"), writes=[gb])
    oacc = P.sb([128, 4, 4096], F32)
    oacc_k = [[Tk() for _ in range(32)] for _ in range(4)]
    for hl in range(4):
        P.op("pool", (lambda h, hl=hl: h.memset(oacc[:, hl, :], 0.0)), (), oacc_k[hl])
    chains = [(hl, d) for hl in range(4) for d in range(2)]
    Sf = {c: [P.sbuf([128, 128], F32) for _ in range(2)] for c in chains}
    Sb = {c: [P.sbuf([128, 128], BF16) for _ in range(2)] for c in chains}
    for c in chains:
        P.memset("pool", Sf[c][0][:], 0.0, writes=[Sf[c][0]])
        P.memset("pool", Sb[c][0][:], 0.0, writes=[Sb[c][0]])
    prod = {c: [dict(P=P.sbuf([128, 128], BF16), WnT=P.sbuf([128, 128], BF16), qdT=P.sbuf([128, 128], BF16),
                     attT=P.sbuf([128, 128], BF16), kt=P.sbuf([128, 128], BF16), v=P.sbuf([128, 128], BF16),
                     glast=P.sbuf([128, 1], F32), vnew=P.sbuf([128, 128], BF16)) for _ in range(2)] for c in chains}

    class Ring:
        def __init__(self, mk, n):
            self.b = [mk() for _ in range(n)]
            self.i = 0

        def get(self):
            x = self.b[self.i % len(self.b)]
            self.i += 1
            return x
    def carve(bank, n):
        out = []
        for i in range(n):
            bb = Buf(bank.t[:, i * 128:(i + 1) * 128], ps=True)
            bb.k = bank.k
            out.append(bb)
        return out
    _pb0 = carve(P.psum([128, 1024], BF16), 8)
    _pb1 = carve(P.psum([128, 1024], BF16), 8)
    _pb = [x for pair in zip(_pb0, _pb1) for x in pair]
    _banks = [carve(P.psum([128, 512], F32), 4) for _ in range(5)]
    _pf = [_banks[b][i] for i in range(4) for b in range(5)]
    psb = Ring(None, 0)
    psb.b = _pb
    psf = Ring(None, 0)
    psf.b = _pf
    pss = P.psum([128, 512], F32)
    tmp = {}
    for c in chains:
        t = {}
        t["qkv"] = [P.sbuf([128, 3, 128], BF16) for _ in range(2)]
        for nm in ("TriG", "X", "DS", "E", "DI"):
            t[nm] = P.sbuf([128, 128], F32)
        for nm in ("ktm", "Ke", "N", "NTt", "IpNT", "Rm", "PT"):
            t[nm] = P.sbuf([128, 128], BF16)
        for nm in ("Xc", "XTc", "Pmc"):
            t[nm] = [P.sbuf([128, 128], BF16) for _ in range(2)]
        for nm in ("ngc", "egc", "lastc", "ktc"):
            t[nm] = P.sbuf([128, 1], F32)
        tmp[c] = t
    evq = [0]

    def evac(out, in_, reads, writes):
        evq[0] += 1
        P.copy("act" if evq[0] % 2 else "dve", out, in_, reads=reads, writes=writes)

    def precompute(stage, c, s, st):
        hl, d = c
        tl = tile_of(d, s)
        pr = prod[c][s % 2]
        t = tmp[c]
        qkv = t["qkv"][s % 2]
        gcol = gb[:, tl, d * 8 + hl:d * 8 + hl + 1]
        bcol = gb[:, tl, d * 8 + 4 + hl:d * 8 + 4 + hl + 1]
        if stage == 0:
            P.dma("sp", qkv[:], qkv_d[hl][:, :, tl * 128:(tl + 1) * 128].rearrange("w p t -> p w t"), writes=[qkv])
            P.act(t["TriG"][:], tri[d], AF.Copy, reads=[cst, gb], writes=[t["TriG"]], scale=gcol)
        elif stage == 1:
            pk, pv = psb.get(), psb.get()
            P.tr(pk[:], qkv[:, 1, :], ident_b[:], reads=[qkv, ident_b], writes=[pk])
            P.tr(pv[:], qkv[:, 2, :], ident_b[:], reads=[qkv, ident_b], writes=[pv])
            evac(t["ktm"][:], pk[:], [pk], [t["ktm"]])
            evac(pr["v"][:], pv[:], [pv], [pr["v"]])
            KK, KQ, EB, gcc = psf.get(), psf.get(), psf.get(), psf.get()
            P.mm(KK[:], [(qkv[:, 1, :], qkv[:, 1, :])], reads=[qkv], writes=[KK])
            P.mm(KQ[:], [(qkv[:, 1, :], qkv[:, 0, :])], reads=[qkv], writes=[KQ])
            P.mm(EB[:], [(ones_f, t["TriG"][:])], reads=[cst, t["TriG"]], writes=[EB])
            P.mm(gcc[:, 0:1], [(tri[d], gcol)], reads=[cst, gb], writes=[gcc])
            last = 127 if d == 0 else 0
            ngc, egc, lastc, ktc = t["ngc"], t["egc"], t["lastc"], t["ktc"]
            P.act(ngc[:], gcc[:, 0:1], AF.Copy, reads=[gcc], writes=[ngc], scale=-1.0)
            P.act(lastc[:], EB[:, last:last + 1], AF.Copy, reads=[EB], writes=[lastc])
            X, DS, E, DI = t["X"], t["DS"], t["E"], t["DI"]
            P.tt("dve", X[:], EB[:], negS[d], ALU.add, reads=[EB, cst], writes=[X])
            P.act(DS[:], X[:], AF.Exp, reads=[X, ngc], writes=[DS], bias=ngc[:, 0:1])
            P.act(E[:], EB[:], AF.Exp, reads=[EB], writes=[E])
            P.act(egc[:], gcc[:, 0:1], AF.Exp, reads=[gcc], writes=[egc])
            P.act(pr["glast"][:], lastc[:], AF.Exp, reads=[lastc], writes=[pr["glast"]])
            P.act(ktc[:], gcc[:, 0:1], AF.Exp, reads=[gcc, lastc], writes=[ktc], scale=-1.0, bias=lastc[:, 0:1])
            P.tt("pool", DI[:], DS[:], ident_f, ALU.add, reads=[DS, cst], writes=[DI])
            N = t["N"]
            P.stt("dve", N[:], KK[:], bcol, DS[:], ALU.mult, ALU.mult, reads=[KK, gb, DS], writes=[N])
            P.tt("dve", pr["attT"][:], KQ[:], DI[:], ALU.mult, reads=[KQ, DI], writes=[pr["attT"]])
            P.tt("pool", pr["qdT"][:], qkv[:, 0, :], E[:], ALU.mult, reads=[qkv, E], writes=[pr["qdT"]])
            P.act(t["Ke"][:], t["ktm"][:], AF.Copy, reads=[t["ktm"], egc], writes=[t["Ke"]], scale=egc[:, 0:1])
            P.act(pr["kt"][:], t["ktm"][:], AF.Copy, reads=[t["ktm"], ktc], writes=[pr["kt"]], scale=ktc[:, 0:1])
            P.tt("pool", t["Pmc"][0][:], ident_b[:], N[:], ALU.subtract, reads=[ident_b, N], writes=[t["Pmc"][0]])
        elif stage == 2:
            N = t["N"]
            pn = psb.get()
            P.tr(pn[:], N[:], ident_b[:], reads=[N, ident_b], writes=[pn])
            evac(t["NTt"][:], pn[:], [pn], [t["NTt"]])
            P.tt("pool", t["IpNT"][:], t["NTt"][:], ident_b[:], ALU.add, reads=[t["NTt"], ident_b], writes=[t["IpNT"]])
        elif 3 <= stage <= 14:
            lev = (stage - 3) // 2 + 1
            half = (stage - 3) % 2
            if lev == 1:
                X, XT = t["N"], t["NTt"]
            else:
                X, XT = t["Xc"][(lev - 1) % 2], t["XTc"][(lev - 1) % 2]
            Pm = t["Pmc"][(lev - 1) % 2]
            XTn = t["XTc"][lev % 2]
            if half == 0:
                p1 = psf.get()
                P.mm(p1[:], [(X[:], XT[:])], reads=[X, XT], writes=[p1])
                evac(XTn[:], p1[:], [p1], [XTn])
                if lev < 6:
                    p2 = psf.get()
                    P.mm(p2[:], [(XT[:], X[:])], reads=[X, XT], writes=[p2])
                    evac(t["Xc"][lev % 2][:], p2[:], [p2], [t["Xc"][lev % 2]])
            else:
                p3 = psf.get()
                P.mm(p3[:], [(XTn[:], Pm[:])], reads=[XTn, Pm], writes=[p3])
                Pn = t["Pmc"][lev % 2]
                P.tt("dve", Pn[:], p3[:], Pm[:], ALU.add, reads=[p3, Pm], writes=[Pn])
        elif stage == 15:
            Pt = t["Pmc"][0]
            pr_ = psf.get()
            P.mm(pr_[:], [(t["IpNT"][:], Pt[:])], reads=[t["IpNT"], Pt], writes=[pr_])
            P.tt("dve", t["Rm"][:], ident_f, pr_[:], ALU.subtract, reads=[cst, pr_], writes=[t["Rm"]])
            ptp = psb.get()
            P.tr(ptp[:], Pt[:], ident_b[:], reads=[Pt, ident_b], writes=[ptp])
            evac(t["PT"][:], ptp[:], [ptp], [t["PT"]])
        elif stage == 16:
            Pt = t["Pmc"][0]
            pc_ = psf.get()
            P.mm(pc_[:], [(t["PT"][:], t["Rm"][:])], reads=[t["PT"], t["Rm"]], writes=[pc_])
            P.tt("dve", pr["P"][:], pc_[:], Pt[:], ALU.add, reads=[pc_, Pt], writes=[pr["P"]])
        elif stage == 17:
            pw = psf.get()
            P.mm(pw[:], [(t["Ke"][:], pr["P"][:])], reads=[t["Ke"], pr["P"]], writes=[pw])
            P.act(pr["WnT"][:], pw[:], AF.Copy, reads=[pw], writes=[pr["WnT"]], scale=-1.0)

    NSTAGE = 18

    def scan(sub, c, s, st):
        hl, d = c
        tl = tile_of(d, s)
        pr = prod[c][s % 2]
        So_f, Sn_f = Sf[c][s % 2], Sf[c][(s + 1) % 2]
        So_b, Sn_b = Sb[c][s % 2], Sb[c][(s + 1) % 2]
        if sub == 0:
            pa = psf.get()
            P.mm(pa[:], [(pr["P"][:], pr["v"][:]), (pr["WnT"][:], So_b[:])], reads=[pr["P"], pr["v"], pr["WnT"], So_b], writes=[pa])
            st["pa"] = pa
        elif sub == 1:
            bcol = gb[:, tl, d * 8 + 4 + hl:d * 8 + 4 + hl + 1]
            P.act(pr["vnew"][:], st["pa"][:], AF.Copy, reads=[st["pa"], gb], writes=[pr["vnew"]], scale=bcol)
        elif sub == 2:
            pso = psf.get()
            if tl >= 2:
                P.mm(pso[:], [(So_b[:], pr["qdT"][:]), (pr["vnew"][:], pr["attT"][:])],
                     reads=[So_b, pr["qdT"], pr["vnew"], pr["attT"]], writes=[pso])
            ps_ = psf.get()
            P.mm(ps_[:], [(pr["kt"][:], pr["vnew"][:])], reads=[pr["kt"], pr["vnew"]], writes=[ps_])
            st["pso"], st["ps"] = pso, ps_
        elif sub == 3:
            P.stt("dve", Sn_b[:], So_f[:], pr["glast"][:, 0:1], st["ps"][:], ALU.mult, ALU.add,
                  reads=[So_f, pr["glast"], st["ps"]], writes=[Sn_b])
            P.stt("dve", Sn_f[:], So_f[:], pr["glast"][:, 0:1], st["ps"][:], ALU.mult, ALU.add,
                  reads=[So_f, pr["glast"], st["ps"]], writes=[Sn_f])
            if tl >= 2:
                n = tl - 2
                osl = oacc[:, hl, n * 128:(n + 1) * 128]
                P.op("dve", (lambda h, osl=osl, pso=st["pso"]: h.tensor_tensor(out=osl, in0=pso[:], in1=osl, op=ALU.add)),
                     [], _ks([st["pso"], oacc_k[hl][n]]))

    pst = {}
    sst = {}
    for s in range(nsteps + 1):
        if s < nsteps:
            for c in chains:
                pst[c] = {}
            for stage in range(NSTAGE):
                for c in chains:
                    precompute(stage, c, s, pst[c])
        if s >= 1:
            for c in chains:
                sst[c] = {}
            for sub in range(4):
                for c in chains:
                    scan(sub, c, s - 1, sst[c])

    g_sq = [P.sbuf([128, 512], F32) for _ in range(2)]
    g_rs = [P.sbuf([128, 512], F32) for _ in range(2)]
    g_z = [P.sbuf([128, 512], BF16) for _ in range(2)]
    g_o = [P.sbuf([128, 512], BF16) for _ in range(2)]
    it = 0
    for hl in range(ngate):
        for gq in range(8):
            sq, rs, zt, og = g_sq[it % 2], g_rs[it % 2], g_z[it % 2], g_o[it % 2]
            it += 1
            oks = oacc_k[hl][gq * 4:(gq + 1) * 4]
            osl = oacc[:, hl, gq * 512:(gq + 1) * 512]
            P.dma("sp", zt[:], zT_d[hl][:, gq * 512:(gq + 1) * 512], writes=[zt])
            P.op("act", (lambda h, sq=sq, osl=osl: h.activation(out=sq[:], in_=osl, func=AF.Square)), _ks(oks), _ks([sq]))
            P.mm(pss[:], [(ones_f, sq[:])], reads=[cst, sq], writes=[pss])
            P.act(rs[:], pss[:], AF.Sqrt, reads=[pss], writes=[rs], scale=1.0 / 128.0, bias=EPS)
            P.op("dve", (lambda h, rs=rs: h.reciprocal(out=rs[:], in_=rs[:])), _ks([rs]), _ks([rs]))
            P.op("dve", (lambda h, rs=rs, osl=osl: h.tensor_tensor(out=rs[:], in0=osl, in1=rs[:], op=ALU.mult)),
                 _ks(oks + [rs]), _ks([rs]))
            P.stt("dve", og[:], rs[:], ogain[:, 0:1], zt[:], ALU.mult, ALU.mult, reads=[rs, ogain, zt], writes=[og])
            P.dma("sp", ogT_out[hl * 128:(hl + 1) * 128, gq * 512:(gq + 1) * 512], og[:], reads=[og])
    P.emit()
    P.close()


NTO = 32


def make_cst2():
    i = np.arange(128)
    ident = np.eye(128, dtype=np.float32)
    tristrict = (i[:, None] < i[None, :]).astype(np.float32)
    ones = np.ones((128, 128), np.float32)
    erow = np.tile((np.arange(16) * 512).astype(np.float32)[None, :], (128, 1))
    pad = np.zeros((128, 112), np.float32)
    return np.ascontiguousarray(np.concatenate([ident, tristrict, ones, erow, pad], axis=1))


def mod_rows(P, nc, cact, ones_f, wmod_ap, bmod_ap, col0, ncols, pbank, out_bc, stage):
    wv = wmod_ap.rearrange("(k p) c -> p k c", p=128)
    P.dma("sp", out_bc[:, 0:ncols], bmod_ap[col0:col0 + ncols].partition_broadcast(128), writes=[out_bc])
    for hh in range(ncols // 512):
        for k in range(8):
            P.dma("sp", stage[:, k, :], wv[:, k, col0 + hh * 512:col0 + (hh + 1) * 512], writes=[stage])
        pb = pbank[hh % len(pbank)]
        P.mm(pb[:], [(cact[:, k, :], stage[:, k, :]) for k in range(8)], reads=[cact, stage], writes=[pb])
        P.tt("dve", out_bc[:, hh * 512:(hh + 1) * 512], pb[:], out_bc[:, hh * 512:(hh + 1) * 512], ALU.add,
             reads=[pb, out_bc], writes=[out_bc])


def load_cact(P, I, cst):
    ones_f = cst[:, 256:384]
    cT = P.sbuf([128, 8], F32)
    P.dma("sp", cT[:], I["cT"][:, :], writes=[cT])
    ca = P.sbuf([128, 8], F32)
    P.act(ca[:], cT[:], AF.Silu, reads=[cT], writes=[ca])
    cactB = P.sbuf([128, 8, 128], F32)
    for k in range(8):
        P.ts("dve", cactB[:, k, :], ones_f, ca[:, k:k + 1], None, ALU.mult, reads=[cst, ca], writes=[cactB])
    return cactB


def c0_inputs(nc):
    d = {}

    def inp(name, shape, dtype=F32):
        d[name] = nc.dram_tensor(name, list(shape), dtype, kind="ExternalInput").ap()
    inp("ogT", [1024, 2048], BF16)
    inp("xo", [2048, 1024])
    inp("cT", [128, 8])
    inp("wmod", [1024, 6144])
    inp("bmod", [6144])
    inp("gffn", [1024])
    inp("wout", [1024, 1024])
    inp("wr", [1024, 16])
    inp("cst2", [128, 512])
    return d


def norm_router(P, I, cst, ident_f, h_tile, A2, B2, wr, rings, aff_sb, u2_out_ap, i):
    junk, ss, rs, t2, u2b, u2T, pT, pl, mx, sm = rings
    P.act(junk[:], h_tile[:], AF.Square, reads=[h_tile], writes=[junk, ss], accum_out=ss[:])
    P.act(rs[:], ss[:], AF.Sqrt, reads=[ss], writes=[rs], scale=1.0 / 1024.0, bias=EPS)
    P.op("dve", (lambda h, rs=rs: h.reciprocal(out=rs[:], in_=rs[:])), _ks([rs]), _ks([rs]))
    P.stt("dve", t2[:], h_tile[:], rs[:, 0:1], A2[:], ALU.mult, ALU.mult, reads=[h_tile, rs, A2], writes=[t2])
    P.tt("dve", t2[:], t2[:], B2[:], ALU.add, reads=[t2, B2], writes=[t2])
    P.copy("act", u2b[:], t2[:], reads=[t2], writes=[u2b])
    P.dma("sp", u2_out_ap, u2b[:], reads=[u2b])
    for hh in range(2):
        def trs(h, hh=hh, t2=t2, pT=pT):
            ins = None
            for k in range(4):
                kk = hh * 4 + k
                ins = h.transpose(out=pT[hh][:, k * 128:(k + 1) * 128], in_=t2[:, kk * 128:(kk + 1) * 128], identity=ident_f)
            return ins
        P.op("pe", trs, _ks([t2, cst]), _ks([pT[hh]]))
        P.copy("act" if hh == 0 else "dve", u2T[:, hh * 512:(hh + 1) * 512], pT[hh][:], reads=[pT[hh]], writes=[u2T])
    P.mm(pl[:, 0:16], [(u2T[:, k * 128:(k + 1) * 128], wr[:, k, :]) for k in range(8)], reads=[u2T, wr], writes=[pl])
    P.op("dve", (lambda h, mx=mx, pl=pl: h.reduce_max(out=mx[:], in_=pl[:, 0:16], axis=AX.X)), [], _ks([pl, mx]))
    P.ts("dve", mx[:], mx[:], -1.0, None, ALU.mult, reads=[mx], writes=[mx])
    P.act(aff_sb[:, i, :], pl[:, 0:16], AF.Exp, reads=[pl, mx], writes=[aff_sb, sm], bias=mx[:, 0:1], accum_out=sm[:])
    P.op("dve", (lambda h, sm=sm: h.reciprocal(out=sm[:], in_=sm[:])), _ks([sm]), _ks([sm]))
    P.ts("dve", aff_sb[:, i, :], aff_sb[:, i, :], sm[:, 0:1], None, ALU.mult, reads=[aff_sb, sm], writes=[aff_sb])


def make_nr_rings(P):
    junk = P.sbuf([128, 1024], BF16)
    ss = P.sbuf([128, 1], F32)
    rs = P.sbuf([128, 1], F32)
    t2 = P.sbuf([128, 1024], F32)
    u2b = P.sbuf([128, 1024], BF16)
    u2T = P.sbuf([128, 1024], F32)
    pT = [P.psum([128, 512], F32) for _ in range(2)]
    pl = P.psum([128, 512], F32)
    mx = P.sbuf([128, 1], F32)
    sm = P.sbuf([128, 1], F32)
    return (junk, ss, rs, t2, u2b, u2T, pT, pl, mx, sm)


def ffn_mod_setup(P, nc, I, cst, cactB, layer, pbank, stage):
    ones_f = cst[:, 256:384]
    base = 0
    B2 = P.sbuf([128, 1024], F32)
    A2 = P.sbuf([128, 1024], F32)
    mod_rows(P, nc, cactB, ones_f, I["wmod"], I["bmod"], 3072, 1024, pbank, B2, stage)
    mod_rows(P, nc, cactB, ones_f, I["wmod"], I["bmod"], 4096, 1024, pbank, A2, stage)
    gf = P.sbuf([128, 1024], F32)
    P.dma("sp", gf[:], I["gffn"].partition_broadcast(128), writes=[gf])
    P.stt("dve", A2[:], A2[:], 1.0, gf[:], ALU.add, ALU.mult, reads=[A2, gf], writes=[A2])
    return A2, B2


def build_c0(nc, I, ogT_ap, x_ap, h1_out, u2_out, aff_out):
    P = PX(nc)
    cst = P.sbuf([128, 512], F32)
    P.dma("sp", cst[:], I["cst2"][:, :], writes=[cst])
    ident_f = cst[:, 0:128]
    cactB = load_cact(P, I, cst)
    pbank = [P.psum([128, 512], F32) for _ in range(2)]
    stage = P.sbuf([128, 8, 512], F32)
    gtm = P.sbuf([128, 1024], F32)
    mod_rows(P, nc, cactB, cst[:, 256:384], I["wmod"], I["bmod"], 2048, 1024, pbank, gtm, stage)
    A2, B2 = ffn_mod_setup(P, nc, I, cst, cactB, 0, pbank, stage)
    wout_b = P.sbuf([128, 8, 1024], BF16)
    wv = I["wout"].rearrange("(k p) c -> p k c", p=128)
    for hh in range(2):
        for k in range(8):
            P.dma("sp", stage[:, k, :], wv[:, k, hh * 512:(hh + 1) * 512], writes=[stage])
        for k in range(8):
            P.copy("pool", wout_b[:, k, hh * 512:(hh + 1) * 512], stage[:, k, :], reads=[stage], writes=[wout_b])
    wr = P.sbuf([128, 8, 16], F32)
    P.dma("sp", wr[:], I["wr"].rearrange("(k p) c -> p k c", p=128), writes=[wr])
    aff_sb = P.sbuf([128, NTO, 16], F32)
    rings = make_nr_rings(P)
    og_ring = [P.sbuf([128, 8, 128], BF16) for _ in range(2)]
    x_ring = [P.sbuf([128, 1024], F32) for _ in range(2)]
    h_ring = [P.sbuf([128, 1024], F32) for _ in range(2)]
    ogv = ogT_ap.rearrange("(k p) t -> p k t", p=128)
    for i in range(NTO):
        og, xt, ht = og_ring[i % 2], x_ring[i % 2], h_ring[i % 2]
        P.dma("sp", og[:], ogv[:, :, i * 128:(i + 1) * 128], writes=[og])
        P.dma("sp", xt[:], x_ap[i * 128:(i + 1) * 128, :], writes=[xt])
        for hh in range(2):
            pb = pbank[hh]
            P.mm(pb[:], [(og[:, k, :], wout_b[:, k, hh * 512:(hh + 1) * 512]) for k in range(8)], reads=[og, wout_b], writes=[pb])
            P.tt("dve", ht[:, hh * 512:(hh + 1) * 512], pb[:], gtm[:, hh * 512:(hh + 1) * 512], ALU.mult,
                 reads=[pb, gtm], writes=[ht])
        P.tt("dve", ht[:], ht[:], xt[:], ALU.add, reads=[ht, xt], writes=[ht])
        P.dma("sp", h1_out[i * 128:(i + 1) * 128, :], ht[:], reads=[ht])
        norm_router(P, I, cst, ident_f, ht, A2, B2, wr, rings, aff_sb, u2_out[i * 128:(i + 1) * 128, :], i)
    P.dma("sp", aff_out.rearrange("(n p) e -> p n e", p=128), aff_sb[:], reads=[aff_sb])
    P.emit()
    P.close()


BIGIDX = 1048576.0


def r_inputs(nc):
    d = {}

    def inp(name, shape, dtype=F32):
        d[name] = nc.dram_tensor(name, list(shape), dtype, kind="ExternalInput").ap()
    inp("affB", [4096, 16])
    inp("affO", [2048, 16])
    inp("u2o", [2048, 1024], BF16)
    inp("slotab", [128, 2])
    inp("cst2", [128, 512])
    return d


def build_r(nc, I, aff_ap, u2_ap, xs_part, idx_out, gate_out):
    P = PX(nc)
    cst = P.sbuf([128, 512], F32)
    P.dma("sp", cst[:], I["cst2"][:, :], writes=[cst])
    ones_b = P.sbuf([128, 128], BF16)
    tri_b = P.sbuf([128, 128], BF16)
    P.copy("dve", ones_b[:], cst[:, 256:384], reads=[cst], writes=[ones_b])
    P.copy("dve", tri_b[:], cst[:, 128:256], reads=[cst], writes=[tri_b])
    erow = cst[:, 384:400]
    affB = P.sbuf([128, 32, 16], F32)
    P.dma("sp", affB[:], aff_ap.rearrange("(n p) e -> p n e", p=128), writes=[affB])
    affO = affB
    u2s = P.sbuf([128, NTO, 1024], BF16)
    for j in range(NTO):
        P.dma("sp", u2s[:, j, :], u2_ap[j * 128:(j + 1) * 128, :], writes=[u2s])
    zks = []
    lo = P.sbuf([128, 16], F32)
    mid = P.sbuf([128, 16], F32)
    cnt = P.sbuf([128, 16], F32)
    inc = P.sbuf([128, 16], F32)
    maskb = P.sbuf([128, 32, 16], BF16)
    pc = P.psum([128, 512], F32)
    P.memset("dve", lo[:], 0.0, writes=[lo])
    for it in range(30):
        step = 2.0 ** -(it + 1)
        P.ts("dve", mid[:], lo[:], step, None, ALU.add, reads=[lo], writes=[mid])
        P.tt("dve", maskb[:], affB[:], mid[:].unsqueeze(1).to_broadcast([128, 32, 16]), ALU.is_ge,
             reads=[affB, mid], writes=[maskb])
        P.mm(pc[:], [(ones_b[:], maskb[:].rearrange("p n e -> p (n e)"))], reads=[ones_b, maskb], writes=[pc])
        P.op("dve", (lambda h: h.tensor_reduce(out=cnt[:], in_=pc[:].rearrange("p (n e) -> p e n", e=16),
                                                axis=AX.X, op=ALU.add)), [], _ks([pc, cnt]))
        P.ts("dve", inc[:], cnt[:], 511.5, step, ALU.is_ge, ALU.mult, reads=[cnt], writes=[inc])
        P.tt("dve", lo[:], lo[:], inc[:], ALU.add, reads=[lo, inc], writes=[lo])
    mo_b = P.sbuf([128, NTO, 16], BF16)
    mo_f = P.sbuf([128, NTO, 16], F32)
    lob = lo[:].unsqueeze(1).to_broadcast([128, NTO, 16])
    P.tt("dve", mo_b[:], affO[:], lob, ALU.is_ge, reads=[affO, lo], writes=[mo_b])
    P.tt("dve", mo_f[:], affO[:], lob, ALU.is_ge, reads=[affO, lo], writes=[mo_f])
    pin = P.psum([128, 512], F32)
    W = NTO * 16
    P.mm(pin[:, 0:W], [(tri_b[:], mo_b[:].rearrange("p n e -> p (n e)"))], reads=[tri_b, mo_b], writes=[pin])
    P.mm(pc[:, 0:W], [(ones_b[:], mo_b[:].rearrange("p n e -> p (n e)"))], reads=[ones_b, mo_b], writes=[pc])
    tot = P.sbuf([128, NTO, 16], F32)
    sa = P.sbuf([128, NTO, 16], F32)
    sb_ = P.sbuf([128, NTO, 16], F32)
    P.copy("dve", tot[:].rearrange("p n e -> p (n e)"), pc[:, 0:W], reads=[pc], writes=[tot])
    P.copy("dve", sa[:], tot[:], reads=[tot], writes=[sa])
    cur, nxt = sa, sb_
    sh = 1
    while sh < NTO:
        P.tt("dve", nxt[:, sh:, :], cur[:, sh:, :], cur[:, :NTO - sh, :], ALU.add, reads=[cur], writes=[nxt])
        P.copy("dve", nxt[:, :sh, :], cur[:, :sh, :], reads=[cur], writes=[nxt])
        cur, nxt = nxt, cur
        sh *= 2
    rank = P.sbuf([128, NTO, 16], F32)
    P.tt("dve", rank[:], cur[:], tot[:], ALU.subtract, reads=[cur, tot], writes=[rank])
    P.tt("dve", rank[:].rearrange("p n e -> p (n e)"), pin[:, 0:W], rank[:].rearrange("p n e -> p (n e)"), ALU.add,
         reads=[pin, rank], writes=[rank])
    sel = P.sbuf([128, NTO, 16], F32)
    P.ts("dve", sel[:], rank[:], 511.5, None, ALU.is_le, reads=[rank], writes=[sel])
    P.tt("dve", sel[:], sel[:], mo_f[:], ALU.mult, reads=[sel, mo_f], writes=[sel])
    slot = P.sbuf([128, NTO, 16], F32)
    P.copy("dve", slot[:], rank[:], reads=[rank], writes=[slot])
    P.tt("dve", slot[:], slot[:], erow.unsqueeze(1).to_broadcast([128, NTO, 16]), ALU.add, reads=[slot, cst], writes=[slot])
    P.ts("dve", slot[:], slot[:], -BIGIDX, None, ALU.add, reads=[slot], writes=[slot])
    P.tt("dve", slot[:], slot[:], sel[:], ALU.mult, reads=[slot, sel], writes=[slot])
    P.ts("dve", slot[:], slot[:], BIGIDX, None, ALU.add, reads=[slot], writes=[slot])
    idx = P.sbuf([128, NTO, 16], U32)
    P.copy("dve", idx[:], slot[:], reads=[slot], writes=[idx])
    gate = P.sbuf([128, NTO, 16], F32)
    P.tt("dve", gate[:], affO[:], sel[:], ALU.mult, reads=[affO, sel], writes=[gate])
    P.dma("sp", idx_out, idx[:], reads=[idx])
    P.dma("sp", gate_out, gate[:], reads=[gate])
    breg = {}

    def bcreg(h):
        if "r" not in breg:
            breg["r"] = h.to_reg(8191)
        return breg["r"]
    for j in range(NTO):
        for e in range(16):
            P.op("pool", (lambda h, j=j, e=e: h.indirect_dma_start(
                out=xs_part[:, :], out_offset=bass.IndirectOffsetOnAxis(ap=idx[:, j, e:e + 1], axis=0),
                in_=u2s[:, j, :], in_offset=None, bounds_check=bcreg(h), oob_is_err=False)),
                _ks([idx, u2s]) + zks, [], dma=True)
    P.emit()
    P.close()


def build_e(nc, cst_ap, wg_ap, wu_ap, wd_ap, xs_ap, ys_out):
    P = PX(nc)
    cst = P.sbuf([128, 512], F32)
    P.dma("sp", cst[:], cst_ap[:, :], writes=[cst])
    ident_b = P.sbuf([128, 128], BF16)
    P.copy("dve", ident_b[:], cst[:, 0:128], reads=[cst], writes=[ident_b])
    units = [dict(wg=P.sbuf([128, 8, 1024], BF16), wu=P.sbuf([128, 8, 1024], BF16), wd=P.sbuf([128, 8, 1024], BF16))
             for _ in range(2)]
    stage = [P.sbuf([128, 2, 1024], F32) for _ in range(4)]
    xa_ring = [P.sbuf([128, 1024], BF16) for _ in range(2)]
    xsT_ring = [P.sbuf([128, 8, 512], BF16) for _ in range(2)]
    hid_ring = [P.sbuf([128, 8, 512], BF16) for _ in range(2)]
    sg_ring = [P.sbuf([128, 512], BF16) for _ in range(2)]
    ys_acc = P.sbuf([128, 4, 1024], F32)
    pg_ring = [P.psum([128, 512], F32) for _ in range(2)]
    pu_ring = [P.psum([128, 512], F32) for _ in range(2)]
    py_ring = [P.psum([128, 512], F32) for _ in range(2)]
    pt = P.psum([128, 1024], BF16)
    sti = [0]

    def chunk_load(U, ci):
        e, h = U // 2, U % 2
        un = units[U % 2]
        which, c2 = ci // 4, ci % 4
        if which == 0:
            src = wg_ap[e].rearrange("(k p) c -> p k c", p=128)[:, 2 * c2:2 * c2 + 2, h * 1024:(h + 1) * 1024]
            dst = un["wg"]
        elif which == 1:
            src = wu_ap[e].rearrange("(k p) c -> p k c", p=128)[:, 2 * c2:2 * c2 + 2, h * 1024:(h + 1) * 1024]
            dst = un["wu"]
        else:
            src = wd_ap[e].rearrange("(k p) c -> p k c", p=128)[:, h * 8 + 2 * c2:h * 8 + 2 * c2 + 2, :]
            dst = un["wd"]
        st = stage[sti[0] % 4]
        sti[0] += 1
        P.dma("sp" if sti[0] % 2 else "pool", st[:], src, writes=[st])
        P.copy("dve" if sti[0] % 2 else "act", dst[:, 2 * c2:2 * c2 + 2, :], st[:], reads=[st], writes=[dst])

    def xs_prep(e):
        xsT = xsT_ring[e % 2]
        for t in range(4):
            xa = xa_ring[t % 2]
            r0 = e * 512 + t * 128
            P.dma("sp", xa[:], xs_ap[r0:r0 + 128, :], writes=[xa])

            def trs(h, xa=xa):
                ins = None
                for k in range(8):
                    ins = h.transpose(out=pt[:, k * 128:(k + 1) * 128], in_=xa[:, k * 128:(k + 1) * 128], identity=ident_b[:])
                return ins
            P.op("pe", trs, _ks([xa, ident_b]), _ks([pt]))
            P.copy("dve" if t % 2 == 0 else "act", xsT[:, :, t * 128:(t + 1) * 128],
                   pt[:].rearrange("p (k t) -> p k t", k=8), reads=[pt], writes=[xsT])

    def compute_steps(U):
        e, h = U // 2, U % 2
        un = units[U % 2]
        xsT = xsT_ring[e % 2]
        hid = hid_ring[U % 2]
        steps = []
        for fc in range(8):
            def st1(fc=fc):
                pg, pu, sg = pg_ring[fc % 2], pu_ring[fc % 2], sg_ring[fc % 2]
                P.mm(pg[:], [(un["wg"][:, k, fc * 128:(fc + 1) * 128], xsT[:, k, :]) for k in range(8)], reads=[un["wg"], xsT], writes=[pg])
                P.mm(pu[:], [(un["wu"][:, k, fc * 128:(fc + 1) * 128], xsT[:, k, :]) for k in range(8)], reads=[un["wu"], xsT], writes=[pu])
                P.act(sg[:], pg[:], AF.Silu, reads=[pg], writes=[sg])
                P.tt("dve", hid[:, fc, :], pu[:], sg[:], ALU.mult, reads=[pu, sg], writes=[hid])
            steps.append(st1)
        for t in range(4):
            for hh in range(2):
                def st2(t=t, hh=hh):
                    py = py_ring[hh]
                    P.mm(py[:], [(hid[:, fc, t * 128:(t + 1) * 128], un["wd"][:, fc, hh * 512:(hh + 1) * 512]) for fc in range(8)],
                         reads=[hid, un["wd"]], writes=[py])
                    if h == 0:
                        P.copy("act", ys_acc[:, t, hh * 512:(hh + 1) * 512], py[:], reads=[py], writes=[ys_acc])
                    else:
                        P.tt("dve", ys_acc[:, t, hh * 512:(hh + 1) * 512], py[:], ys_acc[:, t, hh * 512:(hh + 1) * 512], ALU.add,
                             reads=[py, ys_acc], writes=[ys_acc])
                        if hh == 1:
                            r0 = e * 512 + t * 128
                            P.dma("sp", ys_out[r0:r0 + 128, :], ys_acc[:, t, :], reads=[ys_acc])
                steps.append(st2)
        return steps

    NU = 32
    for ci in range(12):
        chunk_load(0, ci)
    for U in range(NU):
        if U % 2 == 0:
            xs_prep(U // 2)
        steps = compute_steps(U)
        for i, stp in enumerate(steps):
            stp()
            if U + 1 < NU and i < 12:
                chunk_load(U + 1, i)
    P.emit()
    P.close()


def k_inputs(nc):
    d = {}

    def inp(name, shape, dtype=F32):
        d[name] = nc.dram_tensor(name, list(shape), dtype, kind="ExternalInput").ap()
    inp("ysB", [8192, 1024])
    inp("idx", [128, 16, 16], U32)
    inp("gate", [128, 16, 16])
    inp("h1", [2048, 1024])
    inp("cT", [128, 8])
    inp("wmod", [1024, 6144])
    inp("bmod", [6144])
    inp("cst2", [128, 512])
    return d


def combine(P, ys_ap, cst, idx, gate, gtf, h_in_ap, h_out_sink):
    bufs = [P.sbuf([128, 1024], F32) for _ in range(8)]
    for bf in bufs:
        P.memset("pool", bf[:], 0.0, writes=[bf])
    accs = [P.sbuf([128, 1024], F32) for _ in range(2)]
    h_ring = [P.sbuf([128, 1024], F32) for _ in range(2)]
    breg = {}

    def bcreg(h):
        if "r" not in breg:
            breg["r"] = h.to_reg(8191)
        return breg["r"]
    q = 0
    for j in range(NTO):
        acc = accs[j % 2]
        ht = h_ring[j % 2]
        P.dma("sp", ht[:], h_in_ap[j * 128:(j + 1) * 128, :], writes=[ht])
        for e in range(16):
            bf = bufs[q % 8]
            q += 1
            P.op("pool", (lambda h, j=j, e=e, bf=bf: h.indirect_dma_start(
                out=bf[:], out_offset=None, in_=ys_ap[:, :],
                in_offset=bass.IndirectOffsetOnAxis(ap=idx[:, j, e:e + 1], axis=0),
                bounds_check=bcreg(h), oob_is_err=False)), _ks([idx]), _ks([bf]), dma=True)
            if e == 0:
                P.ts("dve", acc[:], bf[:], gate[:, j, e:e + 1], None, ALU.mult, reads=[bf, gate], writes=[acc])
            else:
                P.stt("dve", acc[:], bf[:], gate[:, j, e:e + 1], acc[:], ALU.mult, ALU.add, reads=[bf, gate, acc], writes=[acc])
        P.tt("dve", acc[:], acc[:], gtf[:], ALU.mult, reads=[acc, gtf], writes=[acc])
        P.tt("pool", ht[:], ht[:], acc[:], ALU.add, reads=[ht, acc], writes=[ht])
        h_out_sink(j, ht)


def build_k(nc, I, ys_ap, idx_ap, gate_ap, h_ap, h2_out):
    P = PX(nc)
    cst = P.sbuf([128, 512], F32)
    P.dma("sp", cst[:], I["cst2"][:, :], writes=[cst])
    cactB = load_cact(P, I, cst)
    pbank = [P.psum([128, 512], F32) for _ in range(2)]
    stage = P.sbuf([128, 8, 512], F32)
    gtf = P.sbuf([128, 1024], F32)
    mod_rows(P, nc, cactB, cst[:, 256:384], I["wmod"], I["bmod"], 5120, 1024, pbank, gtf, stage)
    idx = P.sbuf([128, NTO, 16], U32)
    gate = P.sbuf([128, NTO, 16], F32)
    P.dma("sp", idx[:], idx_ap, writes=[idx])
    P.dma("sp", gate[:], gate_ap, writes=[gate])

    def sink(j, ht):
        P.dma("sp", h2_out[j * 128:(j + 1) * 128, :], ht[:], reads=[ht])
    combine(P, ys_ap, cst, idx, gate, gtf, h_ap, sink)
    P.emit()
    P.close()


POOL_W = (2, 4, 8, 16)
POOL_DJ = {2: (-1, 0), 4: (-1, 1), 8: (-2, 2), 16: (-4, 4)}


def pool_ind_list():
    lst = []
    for gi, W in enumerate(POOL_W):
        a, b = POOL_DJ[W]
        for dj in range(a, b + 1):
            lst.append((gi, W, dj))
    return lst


def make_pool_consts():
    lst = pool_ind_list()
    p = np.arange(128)
    ri, ci = p // 64, p % 64
    ind = np.zeros((128, len(lst), 128), np.float32)
    for n, (gi, W, dj) in enumerate(lst):
        dr = 2 * dj + ri[:, None] - ri[None, :]
        dc = ci[:, None] - ci[None, :]
        ind[:, n, :] = ((dr >= -W // 2) & (dr <= W // 2 - 1) & (dc >= -W // 2) & (dc <= W // 2 - 1)).astype(np.float32)
    inv = np.zeros((128, 32, 4), np.float32)
    for io in range(32):
        r = 2 * io + ri
        for gi, W in enumerate(POOL_W):
            cr = np.minimum(r + W - W // 2, 64) - np.maximum(r - W // 2, 0)
            cc = np.minimum(ci + W - W // 2, 64) - np.maximum(ci - W // 2, 0)
            inv[:, io, gi] = 1.0 / (cr * cc).astype(np.float32)
    return ind.astype(ml_dtypes.bfloat16), inv


def build_c1(nc, I, h2_ap, h3_out, u2_out, aff_out):
    P = PX(nc)
    cst = P.sbuf([128, 512], F32)
    P.dma("sp", cst[:], I["cst2"][:, :], writes=[cst])
    ident_f = cst[:, 0:128]
    ones_f = cst[:, 256:384]
    ident_b = P.sbuf([128, 128], BF16)
    P.copy("dve", ident_b[:], ident_f, reads=[cst], writes=[ident_b])
    cactB = load_cact(P, I, cst)
    bankA = P.psum([128, 512], F32)
    bankB = P.psum([128, 512], F32)
    pbank = [bankA, bankB]
    stage = P.sbuf([128, 8, 512], F32)
    B1 = P.sbuf([128, 1024], F32)
    A1 = P.sbuf([128, 1024], F32)
    gtm = P.sbuf([128, 1024], F32)
    tmpv = P.sbuf([128, 1024], F32)
    mod_rows(P, nc, cactB, ones_f, I["wmod"], I["bmod"], 0, 1024, pbank, B1, stage)
    mod_rows(P, nc, cactB, ones_f, I["wmod"], I["bmod"], 1024, 1024, pbank, A1, stage)
    P.dma("sp", tmpv[:], I["gmix"].partition_broadcast(128), writes=[tmpv])
    P.stt("dve", A1[:], A1[:], 1.0, tmpv[:], ALU.add, ALU.mult, reads=[A1, tmpv], writes=[A1])
    mod_rows(P, nc, cactB, ones_f, I["wmod"], I["bmod"], 2048, 1024, pbank, gtm, stage)
    P.dma("sp", tmpv[:], I["pscale"].partition_broadcast(128), reads=[], writes=[tmpv])
    P.tt("dve", gtm[:], gtm[:], tmpv[:], ALU.mult, reads=[gtm, tmpv], writes=[gtm])
    A2, B2 = ffn_mod_setup(P, nc, I, cst, cactB, 1, pbank, stage)
    wr = P.sbuf([128, 8, 16], F32)
    P.dma("sp", wr[:], I["wr"].rearrange("(k p) c -> p k c", p=128), writes=[wr])
    ind = P.sbuf([128, 19, 128], BF16)
    P.dma("sp", ind[:], I["ind"][:, :, :], writes=[ind])
    invc = P.sbuf([128, 32, 4], F32)
    P.dma("sp", invc[:], I["invcnt"][:, :, :], writes=[invc])
    wp_b = P.sbuf([128, 4, 2, 256], BF16)
    wps = P.sbuf([128, 4, 2, 256], F32)
    P.dma("sp", wps[:], I["poolw"].rearrange("g (kk p) e -> p g kk e", p=128), writes=[wps])
    P.copy("pool", wp_b[:], wps[:], reads=[wps], writes=[wp_b])
    u_b = P.sbuf([128, 32, 1024], BF16)
    rs_all = P.sbuf([128, 32], F32)
    hx_ring = [P.sbuf([128, 1024], F32) for _ in range(2)]
    uf_ring = [P.sbuf([128, 1024], F32) for _ in range(2)]
    junk = P.sbuf([128, 1024], BF16)
    ss = P.sbuf([128, 1], F32)
    for i in range(32):
        hx, uf = hx_ring[i % 2], uf_ring[i % 2]
        P.dma("sp", hx[:], h2_ap[i * 128:(i + 1) * 128, :], writes=[hx])
        P.act(junk[:], hx[:], AF.Square, reads=[hx], writes=[junk, ss], accum_out=ss[:])
        P.act(rs_all[:, i:i + 1], ss[:], AF.Sqrt, reads=[ss], writes=[rs_all], scale=1.0 / 1024.0, bias=EPS)
        P.op("dve", (lambda h, i=i: h.reciprocal(out=rs_all[:, i:i + 1], in_=rs_all[:, i:i + 1])), _ks([rs_all]), _ks([rs_all]))
        P.stt("dve", uf[:], hx[:], rs_all[:, i:i + 1], A1[:], ALU.mult, ALU.mult, reads=[hx, rs_all, A1], writes=[uf])
        P.tt("dve", uf[:], uf[:], B1[:], ALU.add, reads=[uf, B1], writes=[uf])
        P.copy("act", u_b[:, i, :], uf[:], reads=[uf], writes=[u_b])
    lst = pool_ind_list()
    aff_sb = P.sbuf([128, NTO, 16], F32)
    rings = make_nr_rings(P)
    pTb = P.psum([128, 1024], BF16)
    py = [P.psum([128, 512], F32) for _ in range(2)]
    p_ring = [P.sbuf([128, 1024], BF16) for _ in range(2)]
    pT_ring = [P.sbuf([128, 1024], BF16) for _ in range(2)]
    h3_ring = [P.sbuf([128, 1024], F32) for _ in range(2)]
    for io in range(32):
        hx, uf, pb_, pTs, h3 = hx_ring[io % 2], uf_ring[io % 2], p_ring[io % 2], pT_ring[io % 2], h3_ring[io % 2]
        P.dma("sp", hx[:], h2_ap[io * 128:(io + 1) * 128, :], writes=[hx])
        P.stt("dve", uf[:], hx[:], rs_all[:, io:io + 1], A1[:], ALU.mult, ALU.mult, reads=[hx, rs_all, A1], writes=[uf])
        P.tt("dve", uf[:], uf[:], B1[:], ALU.add, reads=[uf, B1], writes=[uf])
        for gi in range(4):
            bank = pbank[gi // 2]
            col = (gi % 2) * 256
            pairs = [(ind[:, n, :], u_b[:, io + dj, gi * 256:(gi + 1) * 256]) for n, (g2, W, dj) in enumerate(lst)
                     if g2 == gi and 0 <= io + dj < 32]
            P.mm(bank[:, col:col + 256], pairs, reads=[ind, u_b], writes=[bank])
            P.stt("dve", pb_[:, gi * 256:(gi + 1) * 256], bank[:, col:col + 256], invc[:, io, gi:gi + 1],
                  uf[:, gi * 256:(gi + 1) * 256], ALU.mult, ALU.subtract, reads=[bank, invc, uf], writes=[pb_])

        def trs(h, pb_=pb_):
            ins = None
            for k in range(8):
                ins = h.transpose(out=pTb[:, k * 128:(k + 1) * 128], in_=pb_[:, k * 128:(k + 1) * 128], identity=ident_b[:])
            return ins
        P.op("pe", trs, _ks([pb_, ident_b]), _ks([pTb]))
        P.copy("act", pTs[:], pTb[:], reads=[pTb], writes=[pTs])
        for gi in range(4):
            bank = py[gi // 2]
            col = (gi % 2) * 256
            P.mm(bank[:, col:col + 256], [(pTs[:, (2 * gi + kk) * 128:(2 * gi + kk + 1) * 128], wp_b[:, gi, kk, :]) for kk in range(2)],
                 reads=[pTs, wp_b], writes=[bank])
        for hh in range(2):
            P.tt("dve", h3[:, hh * 512:(hh + 1) * 512], py[hh][:], gtm[:, hh * 512:(hh + 1) * 512], ALU.mult,
                 reads=[py[hh], gtm], writes=[h3])
        P.tt("dve", h3[:], h3[:], hx[:], ALU.add, reads=[h3, hx], writes=[h3])
        P.dma("sp", h3_out[io * 128:(io + 1) * 128, :], h3[:], reads=[h3])
        norm_router(P, I, cst, ident_f, h3, A2, B2, wr, rings, aff_sb, u2_out[io * 128:(io + 1) * 128, :], io)
    P.dma("sp", aff_out.rearrange("(n p) e -> p n e", p=128), aff_sb[:], reads=[aff_sb])
    P.emit()
    P.close()


def kf_inputs(nc):
    d = k_inputs(nc)
    d["fing"] = nc.dram_tensor("fing", [1024], F32, kind="ExternalInput").ap()
    return d


def build_kf(nc, I, ys_ap, idx_ap, gate_ap, h_ap, out_ap):
    P = PX(nc)
    cst = P.sbuf([128, 512], F32)
    P.dma("sp", cst[:], I["cst2"][:, :], writes=[cst])
    cactB = load_cact(P, I, cst)
    pbank = [P.psum([128, 512], F32) for _ in range(2)]
    stage = P.sbuf([128, 8, 512], F32)
    gtf = P.sbuf([128, 1024], F32)
    mod_rows(P, nc, cactB, cst[:, 256:384], I["wmod"], I["bmod"], 5120, 1024, pbank, gtf, stage)
    fg = P.sbuf([128, 1024], F32)
    P.dma("sp", fg[:], I["fing"].partition_broadcast(128), writes=[fg])
    idx = P.sbuf([128, NTO, 16], U32)
    gate = P.sbuf([128, NTO, 16], F32)
    P.dma("sp", idx[:], idx_ap, writes=[idx])
    P.dma("sp", gate[:], gate_ap, writes=[gate])
    junk = P.sbuf([128, 1024], BF16)
    ss = P.sbuf([128, 1], F32)
    rs = P.sbuf([128, 1], F32)
    o_ring = [P.sbuf([128, 1024], F32) for _ in range(2)]

    def sink(j, ht):
        ot = o_ring[j % 2]
        P.act(junk[:], ht[:], AF.Square, reads=[ht], writes=[junk, ss], accum_out=ss[:])
        P.act(rs[:], ss[:], AF.Sqrt, reads=[ss], writes=[rs], scale=1.0 / 1024.0, bias=EPS)
        P.op("dve", (lambda h: h.reciprocal(out=rs[:], in_=rs[:])), _ks([rs]), _ks([rs]))
        P.stt("dve", ot[:], ht[:], rs[:, 0:1], fg[:], ALU.mult, ALU.mult, reads=[ht, rs, fg], writes=[ot])
        P.dma("sp", out_ap[j * 128:(j + 1) * 128, :], ot[:], reads=[ot])
    combine(P, ys_ap, cst, idx, gate, gtf, h_ap, sink)
    P.emit()
    P.close()


GKEYS = ("wqkv", "wz", "wab", "convw", "nalog", "dtb")


def _fused_program():
    nc = bass.Bass("TRN2", target_bir_lowering=False)

    def inp(name, shape, dtype=F32):
        return nc.dram_tensor(name, list(shape), dtype, kind="ExternalInput").ap()

    def scr(name, shape, dtype=F32):
        return nc.dram_tensor(name, list(shape), dtype).ap()
    x = inp("x", [4352, 1024])
    cT = inp("cT", [128, 8, 2])
    cT1 = inp("cT1", [128, 8])
    wmod = [inp("wmod0", [1024, 6144]), inp("wmod1", [1024, 6144])]
    bmod = [inp("bmod0", [6144]), inp("bmod1", [6144])]
    bmodg = inp("bmodg", [128, 16])
    gmixg = inp("gmixg", [128, 8])
    gmix1 = inp("gmix1", [1024])
    gffn = [inp("gffn0", [1024]), inp("gffn1", [1024])]
    ogain = inp("ogain", [128, 1])
    cst = inp("cst", [128, 768])
    cst2 = inp("cst2", [128, 512])
    shapes = {"wqkv": [1024, 1536], "wz": [1024, 512], "wab": [1024, 16], "convw": [128, 12, 5], "nalog": [128, 16], "dtb": [128, 16]}
    grp = [{k: inp(f"{k}{g}", shapes[k]) for k in GKEYS} for g in range(2)]
    wout = inp("wout", [1024, 1024])
    wr = [inp("wr0", [1024, 16]), inp("wr1", [1024, 16])]
    ind = inp("ind", [128, 19, 128], BF16)
    invcnt = inp("invcnt", [128, 32, 4])
    poolw = inp("poolw", [4, 256, 256])
    pscale = inp("pscale", [1024])
    fing = inp("fing", [1024])
    wg = [inp("wg0", [16, 1024, 2048]), inp("wg1", [16, 1024, 2048])]
    wu = [inp("wu0", [16, 1024, 2048]), inp("wu1", [16, 1024, 2048])]
    wd = [inp("wd0", [16, 2048, 1024]), inp("wd1", [16, 2048, 1024])]
    out = nc.dram_tensor("out", [4096, 1024], F32, kind="ExternalOutput").ap()

    qkv_d = scr("qkv_d", [4, 3, 128, 4352], BF16)
    zT_d = scr("zT_d", [4, 128, 4096], BF16)
    gb_d = scr("gb_d", [34, 128, 16])
    ogT_d = scr("ogT_d", [1024, 4096], BF16)
    h1_d = scr("h1_d", [4096, 1024])
    u2_d = scr("u2_d", [4096, 1024], BF16)
    aff_d = scr("aff_d", [4096, 16])
    xs_d = scr("xs_d", [8192, 1024], BF16)
    idx_d = scr("idx_d", [128, 32, 16], U32)
    gate_d = scr("gate_d", [128, 32, 16])
    ys_d = scr("ys_d", [8192, 1024])
    h2_d = scr("h2_d", [4096, 1024])
    h3_d = scr("h3_d", [4096, 1024])

    import os
    PH = int(os.environ.get("FUSED_PH", "99"))
    for g in range(2):
        if g > PH:
            break
        Ig = {"x": x, "cT": cT, "wmod": wmod[0], "bmod": bmodg, "gmix": gmixg, "ogain": ogain, "cst": cst}
        Ig.update(grp[g])
        build_g1(nc, Ig, qkv_d, zT_d, gb_d)
        build_g2(nc, Ig, qkv_d, zT_d, gb_d, ogT_d[g * 512:(g + 1) * 512, :])
    Im = [{"cT": cT1, "wmod": wmod[l], "bmod": bmod[l], "gffn": gffn[l], "wout": wout, "wr": wr[l], "cst2": cst2,
           "gmix": gmix1, "pscale": pscale, "ind": ind, "invcnt": invcnt, "poolw": poolw, "fing": fing} for l in range(2)]
    if PH >= 2:
        build_c0(nc, Im[0], ogT_d, x[256:4352, :], h1_d, u2_d, aff_d)
    if PH >= 3:
        build_r(nc, Im[0], aff_d, u2_d, xs_d, idx_d, gate_d)
    if PH >= 4:
        build_e(nc, cst2, wg[0], wu[0], wd[0], xs_d, ys_d)
    if PH >= 5:
        build_k(nc, Im[0], ys_d, idx_d, gate_d, h1_d, h2_d)
    if PH >= 6:
        build_c1(nc, Im[1], h2_d, h3_d, u2_d, aff_d)
    if PH >= 7:
        build_r(nc, Im[1], aff_d, u2_d, xs_d, idx_d, gate_d)
    if PH >= 8:
        build_e(nc, cst2, wg[1], wu[1], wd[1], xs_d, ys_d)
    if PH >= 9:
        build_kf(nc, Im[1], ys_d, idx_d, gate_d, h3_d, out)
    return nc


def _host_inputs(inp, b):
    d = {}
    g0 = g1_host_inputs(inp, 2 * b)
    g1 = g1_host_inputs(inp, 2 * b + 1)
    d["x"] = g0["x"]
    d["cT"] = g0["cT"]
    d["cT1"] = np.ascontiguousarray(inp["c"][b].reshape(8, 128).T)
    d["bmodg"] = g0["bmod"]
    d["gmixg"] = g0["gmix"]
    d["ogain"] = g0["ogain"]
    d["cst"] = g0["cst"]
    for k in GKEYS:
        d[k + "0"] = g0[k]
        d[k + "1"] = g1[k]
    for l in range(2):
        d[f"wmod{l}"] = np.ascontiguousarray(inp["w_mod"][l])
        d[f"bmod{l}"] = np.ascontiguousarray(inp["b_mod"][l])
        d[f"gffn{l}"] = np.ascontiguousarray(inp["norm_ffn_g"][l])
        d[f"wr{l}"] = np.ascontiguousarray(inp["moe_w_router"][l])
        d[f"wg{l}"] = np.ascontiguousarray(inp["moe_w_gate"][l])
        d[f"wu{l}"] = np.ascontiguousarray(inp["moe_w_up"][l])
        d[f"wd{l}"] = np.ascontiguousarray(inp["moe_w_down"][l])
    d["gmix1"] = np.ascontiguousarray(inp["norm_mix_g"][1])
    d["cst2"] = make_cst2()
    d["wout"] = np.ascontiguousarray(inp["gdn_w_out"][0])
    ind, inv = make_pool_consts()
    d["ind"], d["invcnt"] = ind, inv
    d["poolw"] = np.ascontiguousarray(inp["pool_w"][0])
    d["pscale"] = np.ascontiguousarray(inp["pool_scale"][0])
    d["fing"] = np.ascontiguousarray(inp["final_g"])
    return d


def kernel(**inputs):
    inp = {k: np.asarray(v) for k, v in inputs.items()}
    nc = _fused_program()
    maps = []
    for b in range(4):
        m = _host_inputs(inp, b)
        maps.append(m)
        maps.append(m)
    res = run_bass_kernel_spmd(nc, maps, core_ids=list(range(8))).results
    out = np.stack([np.concatenate([np.asarray(res[2 * b]["out"])[0:2048], np.asarray(res[2 * b + 1]["out"])[2048:4096]], axis=0)
                    for b in range(4)])
    return out.astype(np.float32)
```
